# Optimizing a Trainium2 kernel written in Bass

```python
import math
import jax, jax.numpy as jnp
from jax import lax
import numpy as np

D_MODEL = 1024
BATCH = 32
SEQ = 2048
DEPTH = 1

HEAD_DIM = 64
N_HEADS_DIL = 8
N_HEADS_MOBA = 8
N_HEADS_SELF = N_HEADS_DIL + N_HEADS_MOBA
DIL_WIDTH = N_HEADS_DIL * HEAD_DIM
MOBA_WIDTH = N_HEADS_MOBA * HEAD_DIM
MIX_WIDTH = DIL_WIDTH + MOBA_WIDTH
DILATION_PAIRS = ((128, 1), (512, 4), (2048, 16))
MOBA_BLOCK = 256
MOBA_TOPK = 3
MOBA_QCHUNK = 16
N_BUCKETS = 32
MAX_DISTANCE = 2048
N_MEM = 256
N_HEADS_MEM = 4
HEAD_DIM_MEM = D_MODEL // N_HEADS_MEM
D_FF = 2816
CONV_WIDTH = 3
EPS = 1e-6
NEG_INF = -1e30

kernel_name = "hymba_dilated_moba_convffn_block"


def rmsnorm(x, g):
    xf = x.astype(jnp.float32)
    y = xf * lax.rsqrt(jnp.mean(xf * xf, axis=-1, keepdims=True) + EPS)
    return (y * g.astype(jnp.float32)).astype(x.dtype)


def rel_bucket(dist):
    max_exact = N_BUCKETS // 2
    n = jnp.maximum(dist, 0)
    nf = jnp.maximum(n, 1).astype(jnp.float32)
    large = max_exact + (jnp.log(nf / max_exact) / math.log(MAX_DISTANCE / max_exact)
                         * (N_BUCKETS - max_exact)).astype(jnp.int32)
    large = jnp.minimum(large, N_BUCKETS - 1)
    return jnp.where(n < max_exact, n, large)


def dilated_attention(q, k, v, bias_table):
    B, H, S, hd = q.shape
    scale = hd ** -0.5
    outs, lses = [], []
    for window, dil in DILATION_PAIRS:
        w_sub = window // dil
        L = S // dil
        nblk = -(-L // w_sub)
        Lp = nblk * w_sub

        def to_sub(t):
            t = t.reshape(B, H, L, dil, hd).transpose(0, 1, 3, 2, 4)
            t = jnp.pad(t, ((0, 0), (0, 0), (0, 0), (0, Lp - L), (0, 0)))
            return t.reshape(B, H, dil, nblk, w_sub, hd)

        def with_prev(t):
            prev = jnp.pad(t, ((0, 0), (0, 0), (0, 0), (1, 0), (0, 0), (0, 0)))[:, :, :, :-1]
            return jnp.concatenate([prev, t], axis=4)

        qs = to_sub(q)
        kb = with_prev(to_sub(k))
        vb = with_prev(to_sub(v))
        s = jnp.einsum('bhrnqd,bhrnkd->bhrnqk', qs, kb).astype(jnp.float32) * scale
        i = jnp.arange(w_sub)[:, None]
        j = jnp.arange(2 * w_sub)[None, :]
        diff = w_sub + i - j
        bias = bias_table[:, rel_bucket(diff * dil)].astype(jnp.float32)
        blk = jnp.arange(nblk)[:, None, None]
        valid = (diff >= 0) & (diff <= w_sub) & ((blk - 1) * w_sub + j >= 0)
        s = jnp.where(valid, s + bias[:, None, None], NEG_INF)
        m = jnp.max(s, axis=-1, keepdims=True)
        p = jnp.exp(s - m)
        den = jnp.sum(p, axis=-1, keepdims=True)
        o = jnp.einsum('bhrnqk,bhrnkd->bhrnqd', (p / den).astype(v.dtype), vb)
        lse = (m + jnp.log(den))[..., 0]
        o = o.reshape(B, H, dil, Lp, hd)[:, :, :, :L].transpose(0, 1, 3, 2, 4).reshape(B, H, S, hd)
        lse = lse.reshape(B, H, dil, Lp)[:, :, :, :L].transpose(0, 1, 3, 2).reshape(B, H, S)
        outs.append(o)
        lses.append(lse)
    wts = jax.nn.softmax(jnp.stack(lses), axis=0)
    out = jnp.einsum('pbhs,pbhsd->bhsd', wts, jnp.stack(outs).astype(jnp.float32))
    return out.astype(q.dtype)


def moba_attention(q, k, v, bias_table):
    B, H, S, hd = q.shape
    scale = hd ** -0.5
    nb = -(-S // MOBA_BLOCK)
    Sp = nb * MOBA_BLOCK
    pad = lambda t: jnp.pad(t, ((0, 0), (0, 0), (0, Sp - S), (0, 0)))
    qp, kp, vp = pad(q), pad(k), pad(v)
    kb = kp.reshape(B, H, nb, MOBA_BLOCK, hd)
    vb = vp.reshape(B, H, nb, MOBA_BLOCK, hd)
    k_mean = jnp.mean(kb.astype(jnp.float32), axis=3).astype(q.dtype)
    topk = min(MOBA_TOPK, nb)
    n_chunks = Sp // MOBA_QCHUNK
    qc = qp.reshape(B, H, n_chunks, MOBA_QCHUNK, hd).transpose(2, 0, 1, 3, 4)
    bi = jnp.arange(B)[:, None, None, None]
    hi = jnp.arange(H)[None, :, None, None]
    hi5 = jnp.arange(H)[None, :, None, None, None]
    bias_f = bias_table.astype(jnp.float32)

    def one_chunk(args):
        q_c, c = args
        t = c * MOBA_QCHUNK + jnp.arange(MOBA_QCHUNK)
        own = (c * MOBA_QCHUNK) // MOBA_BLOCK
        gate = jnp.einsum('bhqd,bhnd->bhqn', q_c, k_mean).astype(jnp.float32)
        gate = jnp.where(jnp.arange(nb) < own, gate, NEG_INF)
        _, idx = lax.top_k(gate, topk)
        sel_valid = jnp.arange(topk) < own
        k_sel = kb[bi, hi, idx]
        v_sel = vb[bi, hi, idx]
        s_sel = jnp.einsum('bhqd,bhqnkd->bhqnk', q_c, k_sel).astype(jnp.float32) * scale
        key_pos = idx[..., None] * MOBA_BLOCK + jnp.arange(MOBA_BLOCK)
        dist = t[:, None, None] - key_pos
        s_sel = jnp.where(sel_valid[:, None], s_sel + bias_f[hi5, rel_bucket(dist)], NEG_INF)
        k_own = lax.dynamic_slice_in_dim(kp, own * MOBA_BLOCK, MOBA_BLOCK, axis=2)
        v_own = lax.dynamic_slice_in_dim(vp, own * MOBA_BLOCK, MOBA_BLOCK, axis=2)
        s_own = jnp.einsum('bhqd,bhkd->bhqk', q_c, k_own).astype(jnp.float32) * scale
        dist_own = t[:, None] - (own * MOBA_BLOCK + jnp.arange(MOBA_BLOCK))[None, :]
        s_own = jnp.where(dist_own >= 0, s_own + bias_f[:, rel_bucket(dist_own)][None], NEG_INF)
        nsel = topk * MOBA_BLOCK
        s_all = jnp.concatenate([s_sel.reshape(B, H, MOBA_QCHUNK, nsel), s_own], axis=-1)
        p = jax.nn.softmax(s_all, axis=-1).astype(v.dtype)
        p_sel = p[..., :nsel].reshape(B, H, MOBA_QCHUNK, topk, MOBA_BLOCK)
        o = (jnp.einsum('bhqnk,bhqnkd->bhqd', p_sel, v_sel)
             + jnp.einsum('bhqk,bhkd->bhqd', p[..., nsel:], v_own))
        return o

    out = lax.map(one_chunk, (qc, jnp.arange(n_chunks)))
    return out.transpose(1, 2, 0, 3, 4).reshape(B, H, Sp, hd)[:, :, :S]


def memory_cross_attention(c, m, w_q, w_kv, w_o):
    B, S, D = c.shape
    qm = (c @ w_q).reshape(B, S, N_HEADS_MEM, HEAD_DIM_MEM)
    km, vm = jnp.split(m @ w_kv, 2, axis=-1)
    km = km.reshape(B, N_MEM, N_HEADS_MEM, HEAD_DIM_MEM)
    vm = vm.reshape(B, N_MEM, N_HEADS_MEM, HEAD_DIM_MEM)
    s = jnp.einsum('bqhd,bkhd->bhqk', qm, km).astype(jnp.float32) * (HEAD_DIM_MEM ** -0.5)
    p = jax.nn.softmax(s, axis=-1).astype(c.dtype)
    o = jnp.einsum('bhqk,bkhd->bqhd', p, vm).reshape(B, S, D)
    return o @ w_o


def conv_ffn(f, w_gate, w_up, conv_w, conv_b, w_down):
    S = f.shape[1]
    a = f @ w_gate
    ap = jnp.pad(a, ((0, 0), (CONV_WIDTH - 1, 0), (0, 0)))
    conv = conv_b
    for tap in range(CONV_WIDTH):
        conv = conv + ap[:, tap:tap + S] * conv_w[tap]
    return (jax.nn.silu(conv) * (f @ w_up)) @ w_down


def setup_inputs(seed: int = 0) -> dict:
    key = jax.random.key(seed)
    ks = jax.random.split(key, 20)
    f32 = jnp.float32
    nrm = lambda k, shape, scale: jax.random.normal(k, shape, f32) * scale
    gain = lambda k, shape: 1.0 + 0.02 * jax.random.normal(k, shape, f32)
    return {
        "x": nrm(ks[0], (BATCH, SEQ, D_MODEL), 1.0),
        "mem": nrm(ks[1], (BATCH, N_MEM, D_MODEL), 1.0),
        "w_in": nrm(ks[2], (DEPTH, D_MODEL, 3 * MIX_WIDTH), D_MODEL ** -0.5),
        "g_mix": gain(ks[3], (DEPTH, D_MODEL)),
        "g_out_dil": gain(ks[4], (DEPTH, DIL_WIDTH)),
        "g_out_moba": gain(ks[5], (DEPTH, MOBA_WIDTH)),
        "w_out": nrm(ks[6], (DEPTH, MIX_WIDTH, D_MODEL), MIX_WIDTH ** -0.5),
        "rel_bias": nrm(ks[7], (N_HEADS_SELF, N_BUCKETS), 0.5),
        "g_cross": gain(ks[8], (DEPTH, D_MODEL)),
        "g_mem": gain(ks[9], (DEPTH, D_MODEL)),
        "w_q_mem": nrm(ks[10], (DEPTH, D_MODEL, D_MODEL), D_MODEL ** -0.5),
        "w_kv_mem": nrm(ks[11], (DEPTH, D_MODEL, 2 * D_MODEL), D_MODEL ** -0.5),
        "w_o_mem": nrm(ks[12], (DEPTH, D_MODEL, D_MODEL), D_MODEL ** -0.5),
        "g_ffn": gain(ks[13], (DEPTH, D_MODEL)),
        "w_gate": nrm(ks[14], (DEPTH, D_MODEL, D_FF), D_MODEL ** -0.5),
        "w_up": nrm(ks[15], (DEPTH, D_MODEL, D_FF), D_MODEL ** -0.5),
        "conv_w": nrm(ks[16], (DEPTH, CONV_WIDTH, D_FF), CONV_WIDTH ** -0.5),
        "conv_b": nrm(ks[17], (DEPTH, D_FF), 0.02),
        "w_down": nrm(ks[18], (DEPTH, D_FF, D_MODEL), D_FF ** -0.5),
        "g_final": gain(ks[19], (D_MODEL,)),
    }


def reference(x, mem, w_in, g_mix, g_out_dil, g_out_moba, w_out, rel_bias, g_cross, g_mem,
              w_q_mem, w_kv_mem, w_o_mem, g_ffn, w_gate, w_up, conv_w, conv_b, w_down, g_final):
    B, S, _ = x.shape
    heads = lambda t, n: t.reshape(B, S, n, HEAD_DIM).transpose(0, 2, 1, 3)
    merge = lambda t, w: t.transpose(0, 2, 1, 3).reshape(B, S, w)
    h = x
    for l in range(DEPTH):
        u = rmsnorm(h, g_mix[l])
        qa, ka, va, qb, kb, vb = jnp.split(u @ w_in[l], 6, axis=-1)
        ya = dilated_attention(heads(qa, N_HEADS_DIL), heads(ka, N_HEADS_DIL),
                               heads(va, N_HEADS_DIL), rel_bias[:N_HEADS_DIL])
        yb = moba_attention(heads(qb, N_HEADS_MOBA), heads(kb, N_HEADS_MOBA),
                            heads(vb, N_HEADS_MOBA), rel_bias[N_HEADS_DIL:])
        y = jnp.concatenate([rmsnorm(merge(ya, DIL_WIDTH), g_out_dil[l]),
                             rmsnorm(merge(yb, MOBA_WIDTH), g_out_moba[l])], axis=-1)
        h = h + y @ w_out[l]
        h = h + memory_cross_attention(rmsnorm(h, g_cross[l]), rmsnorm(mem, g_mem[l]),
                                       w_q_mem[l], w_kv_mem[l], w_o_mem[l])
        h = h + conv_ffn(rmsnorm(h, g_ffn[l]), w_gate[l], w_up[l], conv_w[l], conv_b[l], w_down[l])
    return rmsnorm(h, g_final)
```

```python
import math
import numpy as np
import concourse.bass as bass
import concourse.mybir as mybir
from concourse.bass_utils import run_bass_kernel_spmd

F32 = mybir.dt.float32
BF16 = mybir.dt.bfloat16
AF = mybir.ActivationFunctionType
ALU = mybir.AluOpType
AX = mybir.AxisListType

S_LEN = 2048
D = 1024
NT = 16
KC = 8
DFF = 2816
NFC = 22
NMEM = 256
EW = 2176
PITCH = 2304
EPS = 1e-6
NCORES = 8
NB_CORE = 4
SB_BASE = 16640


class Sch:
    CH = 16000
    NDS = 24

    def __init__(self, nc):
        self.nc = nc
        self.E = {'pe': nc.tensor, 'act': nc.scalar, 'dve': nc.vector, 'pool': nc.gpsimd, 'sp': nc.sync}
        self.cnt = {e: 0 for e in self.E}
        self.sems = {e: [] for e in self.E}
        self.seen = {e: {} for e in self.E}
        self.dsem = [nc.alloc_semaphore("dsem%d" % i) for i in range(self.NDS)]
        self.dval = [0] * self.NDS
        self.dnext = {'sp': 0, 'pool': 0}
        self.lastw = {}
        self.rd_e = {}
        self.rd_d = {}
        self.nwaits = 0

    def _sem(self, e, k):
        g = (k - 1) // self.CH
        while len(self.sems[e]) <= g:
            self.sems[e].append(self.nc.alloc_semaphore("s_%s_%d" % (e, len(self.sems[e]))))
        return self.sems[e][g], (k - 1) % self.CH + 1

    def wait(self, e, tok):
        if tok is None:
            return
        if tok[0] == 'e':
            _, e2, k = tok
            if e2 == e and e in ('pe', 'sp'):
                return
            assert k <= self.cnt[e2], ("wait on un-emitted inc", e, tok, self.cnt[e2])
            if self.seen[e].get(('e', e2), 0) >= k:
                return
            sem, v = self._sem(e2, k)
            self.E[e].wait_ge(sem, v)
            self.seen[e][('e', e2)] = k
        else:
            _, s, v = tok
            if self.seen[e].get(('d', s), 0) >= v:
                return
            self.E[e].wait_ge(self.dsem[s], v)
            self.seen[e][('d', s)] = v
        self.nwaits += 1

    def deps(self, e, reads, writes):
        for key in reads:
            self.wait(e, self.lastw.get(key))
        for key in writes:
            self.wait(e, self.lastw.get(key))
            for e2, k in self.rd_e.get(key, {}).items():
                self.wait(e, ('e', e2, k))
            for t in self.rd_d.get(key, ()):
                self.wait(e, t)

    def commit(self, tok, reads, writes):
        for key in reads:
            if tok[0] == 'e':
                d = self.rd_e.setdefault(key, {})
                d[tok[1]] = max(d.get(tok[1], 0), tok[2])
            else:
                self.rd_d.setdefault(key, []).append(tok)
        for key in writes:
            self.lastw[key] = tok
            self.rd_e[key] = {}
            self.rd_d[key] = []

    def op(self, e, fn, reads=(), writes=(), inc=True):
        self.deps(e, reads, writes)
        ins = fn()
        if inc:
            self.cnt[e] += 1
            sem, v = self._sem(e, self.cnt[e])
            ins.then_inc(sem, 1)
            tok = ('e', e, self.cnt[e])
        else:
            tok = ('e', e, self.cnt[e] + 1)
        self.commit(tok, reads, writes)
        return tok

    def dma(self, q, out, in_, reads=(), writes=()):
        half = self.NDS // 2
        i = self.dnext[q]
        self.dnext[q] = (i + 1) % half
        s = i + (half if q == 'pool' else 0)
        if self.dval[s] > 0:
            self.wait(q, ('d', s, self.dval[s]))
        self.deps(q, reads, writes)
        ins = self.E[q].dma_start(out=out, in_=in_)
        self.dval[s] += 16
        ins.then_inc(self.dsem[s], 16)
        tok = ('d', s, self.dval[s])
        self.commit(tok, reads, writes)
        return tok

    def barrier(self, engines=('pe', 'act', 'dve', 'pool')):
        for e2 in engines:
            if e2 != 'dve' and self.cnt[e2] > 0:
                self.wait('dve', ('e', e2, self.cnt[e2]))
        tok = self.op('dve', lambda: self.nc.vector.memset(self.bar[:, :], 0.0), writes=[('bar',)])
        for e in engines:
            if e != 'dve':
                self.wait(e, tok)


def _bucket_table():
    n = np.arange(0, 2048, dtype=np.int64)
    nf = np.maximum(n, 1).astype(np.float32)
    large = 16 + (np.log(nf / np.float32(16)) / np.float32(math.log(128.0)) * np.float32(16)).astype(np.int32)
    large = np.minimum(large, 31)
    return np.where(n < 16, n, large).astype(np.int64)


def host_consts():
    bk = _bucket_table()
    onehot = np.zeros((32, EW), np.float32)
    d = np.arange(2048)
    onehot[bk, d + 128] = 1.0
    mult = np.zeros((2, EW), np.float32)
    mA = (d <= 128).astype(np.float32) + ((d % 4 == 0) & (d <= 512)) + ((d % 16 == 0) & (d <= 2048))
    mult[0, 128:] = mA
    mult[1, 128:] = 1.0
    multr = np.ascontiguousarray(np.broadcast_to(mult[None], (128, 2, EW))).astype(np.float32)
    mob = np.zeros((3, 16, 8), np.float32)
    for qt in range(16):
        own = qt // 2
        for n in range(8):
            mob[0, qt, n] = 0.0 if n < own else -1e30
            mob[1, qt, n] = 1.0 if n < own else 0.0
            mob[2, qt, n] = 1.0 if n == own else 0.0
    mobr = np.ascontiguousarray(np.broadcast_to(mob.reshape(1, -1), (128, 384))).astype(np.float32)
    selhot = np.zeros((128, 8, 128), np.float32)
    for base in (0, 32, 64):
        for n in range(8):
            selhot[base + n, n, :] = 1.0
    return {"c_onehot": onehot, "c_mult": multr.reshape(128, 2 * EW), "c_moba": mobr,
            "c_selhot": selhot.reshape(128, 1024)}


def build_program(NB=NB_CORE, dbg=(), stage=99):
    nc = bass.Bass("TRN2", target_bir_lowering=False)
    S = Sch(nc)
    PE, ACT, DVE, POOL = 'pe', 'act', 'dve', 'pool'

    def din(name, shape, dt=F32):
        return nc.dram_tensor(name, list(shape), dt, kind="ExternalInput")

    x_d = din("x", [NB, S_LEN, D])
    mem_d = din("mem", [NB, NMEM, D])
    w_in_d = din("w_in", [D, 3072])
    w_out_d = din("w_out", [D, D])
    w_q_d = din("w_q_mem", [D, D])
    w_kv_d = din("w_kv_mem", [D, 2 * D])
    w_o_d = din("w_o_mem", [D, D])
    w_gate_d = din("w_gate", [D, DFF])
    w_up_d = din("w_up", [D, DFF])
    w_down_d = din("w_down", [DFF, D])
    g_mix_d = din("g_mix", [D])
    g_cross_d = din("g_cross", [D])
    g_mem_d = din("g_mem", [D])
    g_ffn_d = din("g_ffn", [D])
    g_od_d = din("g_out_dil", [512])
    g_om_d = din("g_out_moba", [512])
    g_fin_d = din("g_final", [D])
    relb_d = din("rel_bias", [16, 32])
    convw_d = din("conv_w", [3, DFF])
    convb_d = din("conv_b", [DFF])
    c_onehot_d = din("c_onehot", [32, EW])
    c_mult_d = din("c_mult", [128, 2 * EW])
    c_moba_d = din("c_moba", [128, 384])
    c_selhot_d = din("c_selhot", [128, 1024])
    out_d = nc.dram_tensor("out", [NB, S_LEN, D], F32, kind="ExternalOutput")
    dbg_t = {}
    for nm in dbg:
        dbg_t[nm] = nc.dram_tensor("dbg_" + nm, [S_LEN, D], F32, kind="ExternalOutput")

    wb_in = nc.dram_tensor("wb_in", [D, 3072], BF16)
    wb_out = nc.dram_tensor("wb_out", [D, D], BF16)
    wb_q = nc.dram_tensor("wb_q", [D, D], BF16)
    wb_kv = nc.dram_tensor("wb_kv", [D, 2 * D], BF16)
    wb_o = nc.dram_tensor("wb_o", [D, D], BF16)
    wb_gate = nc.dram_tensor("wb_gate", [D, DFF], BF16)
    wb_up = nc.dram_tensor("wb_up", [D, DFF], BF16)
    wb_down = nc.dram_tensor("wb_down", [DFF, D], BF16)
    tz_scr = nc.dram_tensor("tz_scr", [16 * 128 * PITCH], BF16)

    def sb(name, shape, dt, off):
        return nc.alloc_sbuf_tensor_at(name, list(shape), dt, offset=SB_BASE + off)

    o = 0
    ident_b = sb("ident_b", [128, 128], BF16, o); o += 256
    ident_f = sb("ident_f", [128, 128], F32, o); o += 512
    selhot = sb("selhot", [128, 8, 128], BF16, o); o += 2048
    mobac = sb("mobac", [128, 3, 16, 8], F32, o); o += 1536
    gfin = sb("gfin", [128, D], F32, o); o += 4096
    convc = sb("convc", [128, NFC, 4], F32, o); o += 352
    gsb = sb("gsb", [128, 5, 8], F32, o); o += 160
    stat = sb("stat", [128, 4, 16], F32, o); o += 256
    staty = sb("staty", [128, 4, 4], F32, o); o += 64
    rden = sb("rden", [128, 8], F32, o); o += 32
    ones8 = sb("ones8", [128, 8], BF16, o); o += 32
    S.bar = sb("bar", [128, 8], F32, o); o += 32
    assert o <= 10240
    WS0 = 10240
    ws = [sb("ws%d" % i, [128, 4096], BF16, WS0 + 8192 * i) for i in range(4)]
    XT0 = WS0 + 32768
    xt = [sb("xt%d" % i, [128, D], F32, XT0 + 4096 * i) for i in range(2)]
    SC0 = XT0 + 8192
    junk = sb("junk", [128, D], BF16, SC0)
    u_tm = [sb("u_tm%d" % i, [128, D], BF16, SC0 + 2048 + 2048 * i) for i in range(2)]
    ybf = [sb("ybf%d" % i, [128, 512], BF16, SC0 + 6144 + 1024 * i) for i in range(2)]
    A2_0 = SC0 + 8192
    uT = sb("uT", [128, KC, S_LEN], BF16, A2_0)
    A1_0 = A2_0 + 32768
    A3_0 = A1_0 + 87744
    assert A3_0 + 32768 + SB_BASE <= 229368
    H = sb("H", [128, NT, D], F32, A1_0)
    o = A1_0
    qkT = sb("qkT", [128, 8, S_LEN], BF16, o); o += 32768
    V = sb("V", [128, NT, 8, 65], BF16, o); o += 16640
    tzb = [sb("tzb%d" % i, [128, 2048], BF16, o + 4096 * i) for i in range(3)]; o += 12288
    ytm = sb("ytm", [128, 4, 512], F32, o); o += 8192
    pexp = [sb("pexp%d" % i, [128, 512], BF16, o + 1024 * i) for i in range(3)]; o += 3072
    pT = [sb("pT%d" % i, [128, 512], BF16, o + 1024 * i) for i in range(3)]; o += 3072
    selT = sb("selT", [128, 4, 512], BF16, o); o += 4096
    selsrc = sb("selsrc", [128, 4, 128], BF16, o); o += 2048
    gm = sb("gm", [128, 32, 8], F32, o); o += 1024
    gm2 = sb("gm2", [128, 32, 8], F32, o); o += 1024
    gsel = sb("gsel", [128, 32, 8], F32, o); o += 1024
    gmx = sb("gmx", [128, 32], F32, o); o += 128
    ksum = sb("ksum", [128, 4, 8], F32, o); o += 128
    kmT = sb("kmT", [128, 4, 8], BF16, o); o += 64
    kmTp = sb("kmTp", [128, 8, 8], BF16, o); o += 128
    assert o <= A3_0, o
    yT = sb("yT", [128, 8, S_LEN], BF16, A3_0)
    o = A3_0
    qmT_t = sb("qmT_t", [128, 8, 512], BF16, o); o += 8192
    o_tm = sb("o_tm", [128, 4, D], BF16, o); o += 8192
    oT_t = sb("oT_t", [128, 8, 512], BF16, o); o += 8192
    pTc = [sb("pTc%d" % i, [128, 512], BF16, o + 1024 * i) for i in range(2)]; o += 2048
    TL0 = A1_0 + 65536
    o = TL0
    kmTc = sb("kmTc", [128, 8, NMEM], BF16, o); o += 4096
    vm = sb("vm", [128, 2, 4, 257], BF16, o); o += 4112 + 16
    mnT = sb("mnT", [128, 8, NMEM], BF16, o); o += 4096
    assert o <= A3_0
    gT = sb("gT", [128, NFC, 1024], BF16, TL0); o = TL0 + 45056
    a_sb = [sb("a_sb%d" % i, [128, 514], F32, o + 2080 * i) for i in range(2)]; o += 4160
    t1 = [sb("t1_%d" % i, [128, 512], F32, o + 2048 * i) for i in range(2)]; o += 4096
    halo = sb("halo", [128, NFC, 2], F32, o); o += 176
    assert o <= A3_0 + 32768, o
    stf = [sb("stf%d" % i, [128, 3072], F32, A1_0 + 12288 * i) for i in range(3)]
    stb = [sb("stb%d" % i, [128, 3072], BF16, A1_0 + 36864 + 6144 * i) for i in range(3)]
    mult_sb = sb("mult_sb", [128, 2, EW], F32, A3_0)
    onehot_sb = sb("onehot_sb", [32, EW], F32, A3_0 + 17408)
    rbT = sb("rbT", [32, 16], F32, A3_0 + 17408 + 8704)
    lh = [sb("lh%d" % i, [32, 128], F32, A3_0 + 17408 + 8704 + 64 + 512 * i) for i in range(2)]
    erow = sb("erow", [128, EW], F32, A2_0)
    e_bf = [sb("e_bf%d" % i, [128, EW], BF16, A2_0 + 8704 + 4352 * i) for i in range(2)]

    ps = [nc.alloc_psum_tensor("ps%d" % i, [128, 512], F32) for i in range(8)]

    def PK(i):
        return ('ps', i)

    rot = {}

    def nxt(name, n):
        v = rot.get(name, 0)
        rot[name] = (v + 1) % n
        return v

    def evac_eng():
        return (ACT, DVE)[nxt('evac', 2)]

    def copy_op(e, out, in_):
        if e == ACT:
            return lambda: nc.scalar.copy(out=out, in_=in_)
        if e == DVE:
            return lambda: nc.vector.tensor_copy(out=out, in_=in_)
        return lambda: nc.gpsimd.tensor_copy(out=out, in_=in_)

    def dump(nm, ap, reads):
        if nm not in dbg_t:
            return
        tok = S.dma('sp', out=dbg_t[nm][:, :].rearrange("(t p) d -> p t d", p=128), in_=ap, reads=reads)
        for e in ('pe', 'act', 'dve', 'pool'):
            S.wait(e, tok)

    out_toks = []

    def finish():
        for tok in out_toks:
            S.wait('sp', tok)
        for s_ in range(S.NDS):
            if S.dval[s_] > 0:
                S.wait('sp', ('d', s_, S.dval[s_]))
        for e in ('pe', 'act', 'dve', 'pool'):
            if S.cnt[e] > 0:
                S.wait('sp', ('e', e, S.cnt[e]))
        return nc, S

    with nc.allow_non_contiguous_dma(reason="tiny one-time parameter loads"):
        for i, gt in enumerate([g_mix_d, g_cross_d, g_mem_d, g_ffn_d]):
            S.dma('sp', out=gsb[:, i, :], in_=bass.AP(gt, 0, [[1, 128], [128, 8]]), writes=[('gsb',)])
        S.dma('sp', out=gsb[:, 4, 0:4], in_=bass.AP(g_od_d, 0, [[1, 128], [128, 4]]), writes=[('gsb',)])
        S.dma('sp', out=gsb[:, 4, 4:8], in_=bass.AP(g_om_d, 0, [[1, 128], [128, 4]]), writes=[('gsb',)])
        for j in range(3):
            S.dma('sp', out=convc[:, :, j], in_=bass.AP(convw_d, j * DFF, [[1, 128], [128, NFC]]), writes=[('convc',)])
        S.dma('sp', out=convc[:, :, 3], in_=bass.AP(convb_d, 0, [[1, 128], [128, NFC]]), writes=[('convc',)])
        S.dma('sp', out=rbT[:, :], in_=bass.AP(relb_d, 0, [[1, 32], [32, 16]]), writes=[('rbT',)])
    S.dma('sp', out=gfin[:, :], in_=bass.AP(g_fin_d, 0, [[0, 128], [1, D]]), writes=[('gfin',)])
    S.dma('sp', out=mobac[:, :, :, :].rearrange("p a b c -> p (a b c)"), in_=c_moba_d[:, :], writes=[('mobac',)])
    S.dma('sp', out=mult_sb[:, :, :].rearrange("p a b -> p (a b)"), in_=c_mult_d[:, :], writes=[('mult',)])
    S.dma('sp', out=onehot_sb[:, :], in_=c_onehot_d[:, :], writes=[('onehot',)])
    S.dma('sp', out=stf[0][:, 0:1024], in_=c_selhot_d[:, :], writes=[('stf', 0)])
    S.op(DVE, lambda: nc.vector.tensor_copy(out=selhot[:, :, :].rearrange("p a b -> p (a b)"), in_=stf[0][:, 0:1024]),
         reads=[('stf', 0)], writes=[('selhot',)])
    S.op(POOL, lambda: nc.gpsimd.memset(ident_b[:, :], 0.0), writes=[('identb',)])
    S.op(POOL, lambda: nc.gpsimd.affine_select(out=ident_b[:, :], in_=ident_b[:, :], pattern=[[-1, 128]],
                                              compare_op=ALU.not_equal, fill=1.0, base=0, channel_multiplier=1),
         reads=[('identb',)], writes=[('identb',)])
    S.op(POOL, lambda: nc.gpsimd.memset(ones8[:, :], 1.0), writes=[('ones8',)])
    S.op(POOL, lambda: nc.gpsimd.memset(ident_f[:, :], 0.0), writes=[('identf',)])
    S.op(POOL, lambda: nc.gpsimd.affine_select(out=ident_f[:, :], in_=ident_f[:, :], pattern=[[-1, 128]],
                                              compare_op=ALU.not_equal, fill=1.0, base=0, channel_multiplier=1),
         reads=[('identf',)], writes=[('identf',)])

    wstep = [0]

    def conv_w(src, K, N, dst, gi):
        for kc in range(K // 128):
            st = wstep[0]
            wstep[0] += 1
            sl = st % 3
            S.dma('sp', out=stf[sl][:, 0:N], in_=src[kc * 128:(kc + 1) * 128, :], writes=[('stf', sl)])
            e = (DVE, ACT)[st % 2]
            o_ap = stb[sl][:, 0:N]
            i_ap = stf[sl][:, 0:N]
            if gi is None:
                fn = copy_op(e, o_ap, i_ap)
                rd = [('stf', sl)]
            else:
                g_ap = gsb[:, gi, kc:kc + 1]
                rd = [('stf', sl), ('gsb',)]
                if e == ACT:
                    fn = lambda o_ap=o_ap, i_ap=i_ap, g_ap=g_ap: nc.scalar.activation(out=o_ap, in_=i_ap, func=AF.Copy, scale=g_ap)
                elif e == DVE:
                    fn = lambda o_ap=o_ap, i_ap=i_ap, g_ap=g_ap: nc.vector.tensor_scalar(out=o_ap, in0=i_ap, scalar1=g_ap, scalar2=None, op0=ALU.mult)
                else:
                    fn = lambda o_ap=o_ap, i_ap=i_ap, g_ap=g_ap: nc.gpsimd.tensor_scalar(out=o_ap, in0=i_ap, scalar1=g_ap, scalar2=None, op0=ALU.mult)
            S.op(e, fn, reads=rd, writes=[('stb', sl)])
            S.dma('pool', out=dst[kc * 128:(kc + 1) * 128, :], in_=stb[sl][:, 0:N], reads=[('stb', sl)], writes=[('wscr',)])

    conv_w(w_in_d, D, 3072, wb_in, 0)
    conv_w(w_out_d, D, D, wb_out, 4)
    conv_w(w_q_d, D, D, wb_q, 1)
    conv_w(w_kv_d, D, 2 * D, wb_kv, 2)
    conv_w(w_o_d, D, D, wb_o, None)
    conv_w(w_gate_d, D, DFF, wb_gate, 3)
    conv_w(w_up_d, D, DFF, wb_up, 3)
    conv_w(w_down_d, DFF, D, wb_down, None)

    for h in range(16):
        grp = 0 if h < 8 else 1
        sl = h % 2
        S.op(DVE, lambda: nc.vector.tensor_copy(out=lh[sl][:, :], in_=rbT[:, h:h + 1].to_broadcast([32, 128])),
             reads=[('rbT',)], writes=[('lh', sl)])
        for j in range(5):
            c0 = j * 512
            w = min(512, EW - c0)
            S.op(PE, lambda: nc.tensor.matmul(ps[j][:, 0:w], lh[sl][:, :], onehot_sb[:, c0:c0 + w], start=True, stop=True),
                 reads=[('lh', sl), ('onehot',)], writes=[PK(j)])
            S.op(ACT, lambda: nc.scalar.activation(out=erow[:, c0:c0 + w], in_=ps[j][:, 0:w], func=AF.Exp),
                 reads=[PK(j)], writes=[('erow', j)])
            S.op(DVE, lambda: nc.vector.tensor_tensor(out=e_bf[sl][:, c0:c0 + w], in0=erow[:, c0:c0 + w],
                                                      in1=mult_sb[:, grp, c0:c0 + w], op=ALU.mult),
                 reads=[('erow', j), ('mult',)], writes=[('e_bf', sl)])
        S.dma('pool', out=bass.AP(tz_scr, h * 128 * PITCH, [[PITCH + 1, 128], [1, EW]]), in_=e_bf[sl][:, :],
              reads=[('e_bf', sl)], writes=[('tzscr',)])

    for e in ('sp', 'pool', 'pe', 'act', 'dve'):
        S.wait(e, S.lastw.get(('wscr',)))
        S.wait(e, S.lastw.get(('tzscr',)))
        for s_ in range(S.NDS):
            if S.dval[s_] > 0:
                S.wait(e, ('d', s_, S.dval[s_]))
    S.barrier()

    def rstd_from_ss(ss_ap, v_ap, r_ap, n_feat, keys_in, key_out):
        S.op(DVE, lambda: nc.vector.tensor_scalar(out=v_ap, in0=ss_ap, scalar1=1.0 / n_feat, scalar2=EPS,
                                                  op0=ALU.mult, op1=ALU.add), reads=keys_in, writes=[key_out + ('v',)])
        S.op(ACT, lambda: nc.scalar.activation(out=v_ap, in_=v_ap, func=AF.Ln), reads=[key_out + ('v',)], writes=[key_out + ('v',)])
        S.op(ACT, lambda: nc.scalar.activation(out=r_ap, in_=v_ap, func=AF.Exp, scale=-0.5), reads=[key_out + ('v',)], writes=[key_out])

    def norm_tile(src_ap, src_keys, dstT, col0, dst_keys, idx):
        S.op(ACT, lambda: nc.scalar.activation(out=junk[:, :], in_=src_ap, func=AF.Square, accum_out=stat[:, 0, idx:idx + 1]),
             reads=src_keys, writes=[('junk',), ('ss', idx)])
        rstd_from_ss(stat[:, 0, idx:idx + 1], stat[:, 1, idx:idx + 1], stat[:, 2, idx:idx + 1], D, [('ss', idx)], ('rstd', idx))
        us = nxt('u_tm', 2)
        S.op(POOL, lambda: nc.gpsimd.tensor_scalar(out=u_tm[us][:, :], in0=src_ap, scalar1=stat[:, 2, idx:idx + 1], scalar2=None, op0=ALU.mult),
             reads=list(src_keys) + [('rstd', idx)], writes=[('u_tm', us)])
        tpb = ps[5][:, :].bitcast(BF16)
        for kc in range(KC):
            S.op(PE, lambda: nc.tensor.transpose(tpb[:, kc * 128:(kc + 1) * 128], u_tm[us][:, kc * 128:(kc + 1) * 128], ident_b[:, :]),
                 reads=[('u_tm', us), ('identb',)], writes=[PK(5)], inc=(kc == KC - 1))
        e = evac_eng()
        S.op(e, copy_op(e, dstT[:, :, col0:col0 + 128], tpb[:, 0:1024].rearrange("p (a b) -> p a b", a=KC)),
             reads=[PK(5)], writes=dst_keys)

    def load_w(scr, ncols_total, col0, ncols, row0=0, nkc=KC):
        s = nxt('ws', 4)
        view = ws[s][:, 0:nkc * ncols].rearrange("p (a b) -> p a b", a=nkc)
        S.dma('sp', out=view, in_=bass.AP(scr, row0 * ncols_total + col0, [[ncols_total, 128], [128 * ncols_total, nkc], [1, ncols]]),
              writes=[('ws', s)])
        return s, view

    if stage == 0:
        return finish()
    for b in range(NB):
        S.barrier()
        for t in range(NT):
            xs = nxt('xt', 2)
            S.dma('pool', out=xt[xs][:, :], in_=x_d[b, t * 128:(t + 1) * 128, :], writes=[('xt', xs)])
            norm_tile(xt[xs][:, :], [('xt', xs)], uT, t * 128, [('uT', t)], t)

        if stage == 1:
            dump('h1', uT[:, :, :].bitcast(F32).rearrange("p a (b c) -> p (a b) c", c=1024)[:, :, :] if False else H[:, :, :], [])
            return finish()
        S.op(POOL, lambda: nc.gpsimd.memset(V[:, :, :, 64:65], 1.0), writes=[('Vones',)])
        S.op(POOL, lambda: nc.gpsimd.memset(selsrc[:, :, :], 0.0), writes=[('selsrc',)])

        for g in range(2):
            cb = g * 1536
            sq, wq = load_w(wb_in, 3072, cb, 512)
            sk, wk = load_w(wb_in, 3072, cb + 512, 512)
            sv, wv = load_w(wb_in, 3072, cb + 1024, 512)
            for c in range(8):
                wsl, wvw = (sq, wq) if c < 4 else (sk, wk)
                for tt in range(4):
                    pb = nxt('psS', 3)
                    for kc in range(KC):
                        S.op(PE, lambda: nc.tensor.matmul(ps[pb][:, :], wvw[:, kc, (c % 4) * 128:(c % 4 + 1) * 128],
                                                          uT[:, kc, tt * 512:(tt + 1) * 512], start=(kc == 0), stop=(kc == KC - 1)),
                             reads=[('ws', wsl)] + [('uT', 4 * tt + i) for i in range(4)], writes=[PK(pb)], inc=(kc == KC - 1))
                    e = evac_eng()
                    S.op(e, copy_op(e, qkT[:, c, tt * 512:(tt + 1) * 512], ps[pb][:, :]), reads=[PK(pb)], writes=[('qk', c, tt)])
                    if g == 1 and c >= 4:
                        S.op(DVE, lambda: nc.vector.tensor_reduce(out=ksum[:, c - 4, tt * 2:(tt + 1) * 2],
                                                                  in_=qkT[:, c, tt * 512:(tt + 1) * 512].rearrange("p (a b) -> p a b", a=2), axis=AX.X, op=ALU.add),
                             reads=[('qk', c, tt)], writes=[('ksum',)])
            for t in range(NT):
                pb = nxt('psS', 3)
                for kc in range(KC):
                    S.op(PE, lambda: nc.tensor.matmul(ps[pb][:, :], uT[:, kc, t * 128:(t + 1) * 128], wv[:, kc, :],
                                                      start=(kc == 0), stop=(kc == KC - 1)),
                         reads=[('ws', sv), ('uT', t)], writes=[PK(pb)], inc=(kc == KC - 1))
                e = evac_eng()
                S.op(e, copy_op(e, V[:, t, :, 0:64], ps[pb][:, :].rearrange("p (a b) -> p a b", a=8)),
                     reads=[PK(pb), ('Vones',)], writes=[('V', t)])
            if g == 1:
                S.op(DVE, lambda: nc.vector.memset(kmTp[:, :, :], 0.0), writes=[('kmT',)])
                for hp_ in range(2):
                    S.op(DVE, lambda: nc.vector.tensor_scalar(
                        out=kmTp[hp_ * 64:(hp_ + 1) * 64, :, :].rearrange("p (a two) n -> p a two n", two=2)[:, :, hp_, :],
                        in0=ksum[hp_ * 64:(hp_ + 1) * 64, :, :], scalar1=1.0 / 256.0, scalar2=None, op0=ALU.mult),
                         reads=[('ksum',), ('kmT',)], writes=[('kmT',)])

            if stage == 2:
                return finish()
            if stage == 26 and g == 1:
                return finish()
            for qj in range(4):
                q0 = qj * 512
                if g == 1:
                    for sub in range(4):
                        qt = qj * 4 + sub
                        for h in range(8):
                            hp = h % 2
                            S.op(PE, lambda: nc.tensor.matmul(ps[6][:, sub * 64 + h * 8: sub * 64 + h * 8 + 8],
                                                              qkT[:, h // 2, qt * 128:(qt + 1) * 128],
                                                              kmTp[:, h, :], start=True, stop=True),
                                 reads=[('qk', h // 2, qj), ('kmT',)], writes=[PK(6)], inc=(sub == 3 and h == 7))
                    g4 = lambda t_: t_[:, :, :].rearrange("p (s h) n -> p s h n", s=4)
                    pmask = lambda k_: mobac[:, k_, qj * 4:(qj + 1) * 4, :].unsqueeze(2).to_broadcast([128, 4, 8, 8])
                    S.op(DVE, lambda: nc.vector.tensor_tensor(out=g4(gm), in0=ps[6][:, 0:256].rearrange("p (s h n) -> p s h n", s=4, h=8),
                                                              in1=pmask(0), op=ALU.add), reads=[PK(6), ('mobac',)], writes=[('gm',)])
                    src = gm
                    for it in range(3):
                        S.op(DVE, lambda: nc.vector.tensor_reduce(out=gmx[:, :], in_=src[:, :, :], axis=AX.X, op=ALU.max),
                             reads=[('gm',), ('gm2',)], writes=[('gmx',)])
                        if it < 2:
                            S.op(DVE, lambda: nc.vector.tensor_tensor(out=gsel[:, :, :], in0=src[:, :, :],
                                                                      in1=gmx[:, :].unsqueeze(2).to_broadcast([128, 32, 8]), op=ALU.is_ge),
                                 reads=[('gm',), ('gm2',), ('gmx',)], writes=[('gsel',)])
                            S.op(DVE, lambda: nc.vector.scalar_tensor_tensor(out=gm2[:, :, :], in0=gsel[:, :, :], scalar=-1e30, in1=src[:, :, :],
                                                                             op0=ALU.mult, op1=ALU.add),
                                 reads=[('gsel',), ('gm',), ('gm2',)], writes=[('gm2',)])
                            src = gm2
                    S.op(DVE, lambda: nc.vector.tensor_tensor(out=gsel[:, :, :], in0=gm[:, :, :],
                                                              in1=gmx[:, :].unsqueeze(2).to_broadcast([128, 32, 8]), op=ALU.is_ge),
                         reads=[('gm',), ('gmx',)], writes=[('gsel',)])
                    S.op(DVE, lambda: nc.vector.tensor_tensor(out=g4(gsel), in0=g4(gsel), in1=pmask(1), op=ALU.mult),
                         reads=[('gsel',), ('mobac',)], writes=[('gsel',)])
                    S.op(DVE, lambda: nc.vector.tensor_tensor(out=g4(gsel), in0=g4(gsel), in1=pmask(2), op=ALU.add),
                         reads=[('gsel',), ('mobac',)], writes=[('gsel',)])
                    for sub in range(4):
                        for gi in range(4):
                            S.op(DVE, lambda: nc.vector.tensor_scalar(
                                out=selsrc[:, gi, :].rearrange("p (h c) -> p h c", c=64)[:, 0:2, 0:8],
                                in0=gsel[:, sub * 8 + gi * 2: sub * 8 + gi * 2 + 2, :], scalar1=32768.0, scalar2=-32768.0,
                                op0=ALU.mult, op1=ALU.add), reads=[('gsel',), ('selsrc',)], writes=[('selsrc',)])
                        tp7 = ps[7][:, :].bitcast(BF16)
                        for gi in range(4):
                            S.op(PE, lambda: nc.tensor.transpose(tp7[0:96, gi * 128:(gi + 1) * 128], selsrc[:, gi, 0:96], ident_b[:, :]),
                                 reads=[('selsrc',), ('identb',)], writes=[PK(7)], inc=(gi == 3))
                        e = evac_eng()
                        S.op(e, copy_op(e, selT[0:96, :, sub * 128:(sub + 1) * 128], tp7[0:96, 0:512].rearrange("p (a b) -> p a b", a=4)),
                             reads=[PK(7)], writes=[('selT',)])

                if stage == 27 and g == 1:
                    return finish()
                nk = 4 * qj + 4
                W = (qj + 1) * 512
                hstate = {}
                info = []

                def emit_pv(i):
                    h, kt, qs, N, sl = info[i]
                    tzs_, pvb, pv3 = hstate[h]
                    nsub = N // 128
                    for s_ in range(nsub):
                        subq = (qs - q0) // 128 + s_
                        S.op(PE, lambda: nc.tensor.matmul(pv3[:, subq, :], pT[sl][:, s_ * 128:(s_ + 1) * 128], V[:, kt, h, :],
                                                          start=(kt == 0 and subq == 0), stop=(kt == 4 * qj + 3 and subq == 3),
                                                          skip_group_check=True),
                             reads=[('pT', sl), ('V', kt), ('Vones',)], writes=[PK(pvb)], inc=(s_ == nsub - 1))
                    if kt == nk - 1:
                        S.op(DVE, lambda: nc.vector.reciprocal(out=rden[:, 0:4], in_=pv3[:, :, 64]), reads=[PK(pvb)], writes=[('rden',)])
                        S.op(DVE, lambda: nc.vector.tensor_tensor(out=ytm[:, :, h * 64:(h + 1) * 64], in0=pv3[:, :, 0:64],
                                                                  in1=rden[:, 0:4].unsqueeze(2).to_broadcast([128, 4, 64]), op=ALU.mult),
                             reads=[PK(pvb), ('rden',)], writes=[('ytm',)])

                for h in range(8):
                    hp = h % 2
                    tzs = nxt('tz', 3)
                    S.dma('pool', out=tzb[tzs][:, 0:W],
                          in_=bass.AP(tz_scr, (g * 8 + h) * 128 * PITCH + 128, [[PITCH, 128], [1, W]]), writes=[('tz', tzs)])
                    pvb_ = 3 + nxt('pv', 2)
                    hstate[h] = (tzs, pvb_, ps[pvb_][:, 0:260].rearrange("p (s d) -> p s d", d=65))
                    for kt in range(nk):
                        Dd = q0 - kt * 128
                        if Dd >= 0:
                            qs, N, c0 = q0, 512, Dd
                        else:
                            qs, N, c0 = kt * 128, 512 + Dd, 0
                        sl = nxt('pslot', 3)
                        info.append((h, kt, qs, N, sl))
                        i = len(info) - 1
                        sbk = nxt('psS', 3)
                        S.op(PE, lambda: nc.tensor.matmul(ps[sbk][:, 0:N], qkT[hp * 64:(hp + 1) * 64, 4 + h // 2, kt * 128:(kt + 1) * 128],
                                                          qkT[hp * 64:(hp + 1) * 64, h // 2, qs:qs + N], start=True, stop=(g == 0)),
                             reads=[('qk', 4 + h // 2, kt // 4), ('qk', h // 2, qj)], writes=[PK(sbk)], inc=(g == 0))
                        if g == 1:
                            gi = h // 2
                            S.op(PE, lambda: nc.tensor.matmul(ps[sbk][:, 0:N], selhot[hp * 64:hp * 64 + 32, kt // 2, :],
                                                              selT[hp * 64:hp * 64 + 32, gi, qs - q0:qs - q0 + N], start=False, stop=True),
                                 reads=[('selhot',), ('selT',)], writes=[PK(sbk)], inc=True)
                        S.op(ACT, lambda: nc.scalar.activation(out=pexp[sl][:, 0:N], in_=ps[sbk][:, 0:N], func=AF.Exp, scale=0.125),
                             reads=[PK(sbk)], writes=[('pexp', sl)])
                        S.op(DVE, lambda: nc.vector.tensor_tensor(out=pT[sl][:, 0:N], in0=pexp[sl][:, 0:N], in1=tzb[tzs][:, c0:c0 + N], op=ALU.mult),
                             reads=[('pexp', sl), ('tz', tzs)], writes=[('pT', sl)])
                        if i > 1:
                            emit_pv(i - 2)
                emit_pv(len(info) - 2)
                emit_pv(len(info) - 1)

                if stage == 28 and g == 1 and qj == 0:
                    return finish()
                for sub in range(4):
                    S.op(ACT, lambda: nc.scalar.activation(out=junk[:, 0:512], in_=ytm[:, sub, :], func=AF.Square, accum_out=staty[:, 0, sub:sub + 1]),
                         reads=[('ytm',)], writes=[('junk',), ('ssy',)])
                rstd_from_ss(staty[:, 0, :], staty[:, 1, :], staty[:, 2, :], 512, [('ssy',)], ('rstdy',))
                for sub in range(4):
                    t = qj * 4 + sub
                    ys = nxt('ybf', 2)
                    S.op(POOL, lambda: nc.gpsimd.tensor_scalar(out=ybf[ys][:, :], in0=ytm[:, sub, :], scalar1=staty[:, 2, sub:sub + 1], scalar2=None, op0=ALU.mult),
                         reads=[('ytm',), ('rstdy',)], writes=[('ybf', ys)])
                    tpb = ps[5][:, :].bitcast(BF16)
                    for kc in range(4):
                        S.op(PE, lambda: nc.tensor.transpose(tpb[:, kc * 128:(kc + 1) * 128], ybf[ys][:, kc * 128:(kc + 1) * 128], ident_b[:, :]),
                             reads=[('ybf', ys), ('identb',)], writes=[PK(5)], inc=(kc == 3))
                    e = evac_eng()
                    S.op(e, copy_op(e, yT[:, g * 4:(g + 1) * 4, t * 128:(t + 1) * 128], tpb[:, 0:512].rearrange("p (a b) -> p a b", a=4)),
                         reads=[PK(5)], writes=[('yT', g, t)])
                if stage == 29 and g == 1 and qj == 0:
                    return finish()

            if stage == 25 and g == 0:
                return finish()
        if stage == 3:
            return finish()
        S.barrier()
        if stage == 37:
            return finish()
        so0, wo0 = load_w(wb_out, D, 0, 512)
        so1, wo1 = load_w(wb_out, D, 512, 512)
        for t in range(NT):
            xs = nxt('xt', 2)
            S.dma('pool', out=xt[xs][:, :], in_=x_d[b, t * 128:(t + 1) * 128, :], writes=[('xt', xs)])
            for nh in range(2):
                wsl, wvw = (so0, wo0) if nh == 0 else (so1, wo1)
                pb = nxt('psS', 3)
                for kc in range(KC):
                    S.op(PE, lambda: nc.tensor.matmul(ps[pb][:, :], yT[:, kc, t * 128:(t + 1) * 128], wvw[:, kc, :],
                                                      start=(kc == 0), stop=(kc == KC - 1)),
                         reads=[('ws', wsl), ('yT', 0, t), ('yT', 1, t)], writes=[PK(pb)], inc=(kc == KC - 1))
                S.op(DVE, lambda: nc.vector.tensor_tensor(out=H[:, t, nh * 512:(nh + 1) * 512], in0=ps[pb][:, :],
                                                          in1=xt[xs][:, nh * 512:(nh + 1) * 512], op=ALU.add),
                     reads=[PK(pb), ('xt', xs)], writes=[('H', t)])
            if stage != 35:
                norm_tile(H[:, t, :], [('H', t)], uT, t * 128, [('uT', t)], t)
        dump('h1', H[:, :, :], [('H', t) for t in range(NT)])
        if stage == 35:
            return finish()
        if stage == 4:
            return finish()
        S.barrier()

        for m in range(2):
            xs = nxt('xt', 2)
            S.dma('pool', out=xt[xs][:, :], in_=mem_d[b, m * 128:(m + 1) * 128, :], writes=[('xt', xs)])
            norm_tile(xt[xs][:, :], [('xt', xs)], mnT, m * 128, [('mnT', m)], m)
        S.op(POOL, lambda: nc.gpsimd.memset(vm[:, :, :, 256:257], 1.0), writes=[('vmones',)])
        wks = [load_w(wb_kv, 2 * D, j * 512, 512) for j in range(4)]
        for c in range(8):
            wsl, wvw = wks[c // 4]
            pb = nxt('psS', 3)
            for kc in range(KC):
                S.op(PE, lambda: nc.tensor.matmul(ps[pb][:, 0:NMEM], wvw[:, kc, (c % 4) * 128:(c % 4 + 1) * 128], mnT[:, kc, :],
                                                  start=(kc == 0), stop=(kc == KC - 1)),
                     reads=[('ws', wsl), ('mnT', 0), ('mnT', 1)], writes=[PK(pb)], inc=(kc == KC - 1))
            e = evac_eng()
            S.op(e, copy_op(e, kmTc[:, c, :], ps[pb][:, 0:NMEM]), reads=[PK(pb)], writes=[('kmTc', c)])
        for m in range(2):
            for nh in range(2):
                wsl, wvw = wks[2 + nh]
                pb = nxt('psS', 3)
                for kc in range(KC):
                    S.op(PE, lambda: nc.tensor.matmul(ps[pb][:, :], mnT[:, kc, m * 128:(m + 1) * 128], wvw[:, kc, :],
                                                      start=(kc == 0), stop=(kc == KC - 1)),
                         reads=[('ws', wsl), ('mnT', m)], writes=[PK(pb)], inc=(kc == KC - 1))
                e = evac_eng()
                S.op(e, copy_op(e, vm[:, m, 2 * nh:2 * nh + 2, 0:256], ps[pb][:, :].rearrange("p (a b) -> p a b", a=2)),
                     reads=[PK(pb), ('vmones',)], writes=[('vm', m)])
        wqs = [load_w(wb_q, D, j * 512, 512) for j in range(2)]
        wos = [load_w(wb_o, D, j * 512, 512) for j in range(2)]
        for qj in range(4):
            q0 = qj * 512
            for c in range(8):
                wsl, wvw = wqs[c // 4]
                pb = nxt('psS', 3)
                for kc in range(KC):
                    S.op(PE, lambda: nc.tensor.matmul(ps[pb][:, :], wvw[:, kc, (c % 4) * 128:(c % 4 + 1) * 128], uT[:, kc, q0:q0 + 512],
                                                      start=(kc == 0), stop=(kc == KC - 1)),
                         reads=[('ws', wsl)] + [('uT', 4 * qj + i) for i in range(4)], writes=[PK(pb)], inc=(kc == KC - 1))
                e = evac_eng()
                S.op(e, copy_op(e, qmT_t[:, c, :], ps[pb][:, :]), reads=[PK(pb)], writes=[('qmT', c)])
            for hm in range(4):
                for m in range(2):
                    pb = nxt('psS', 3)
                    for dc in range(2):
                        S.op(PE, lambda: nc.tensor.matmul(ps[pb][:, :], kmTc[:, 2 * hm + dc, m * 128:(m + 1) * 128], qmT_t[:, 2 * hm + dc, :],
                                                          start=(dc == 0), stop=(dc == 1)),
                             reads=[('kmTc', 2 * hm + dc), ('qmT', 2 * hm + dc)], writes=[PK(pb)], inc=(dc == 1))
                    S.op(ACT, lambda: nc.scalar.activation(out=pTc[m][:, :], in_=ps[pb][:, :], func=AF.Exp, scale=1.0 / 16.0),
                         reads=[PK(pb)], writes=[('pTc', m)])
                for sub in range(4):
                    for m in range(2):
                        S.op(PE, lambda: nc.tensor.matmul(ps[3 + sub // 2][:, (sub % 2) * 256:(sub % 2 + 1) * 256],
                                                          pTc[m][:, sub * 128:(sub + 1) * 128], vm[:, m, hm, 0:256],
                                                          start=(m == 0 and sub % 2 == 0), stop=(m == 1 and sub % 2 == 1), skip_group_check=True),
                             reads=[('pTc', m), ('vm', m)], writes=[PK(3 + sub // 2)], inc=False)
                        S.op(PE, lambda: nc.tensor.matmul(ps[6][:, sub * 8:(sub + 1) * 8], pTc[m][:, sub * 128:(sub + 1) * 128], ones8[:, :],
                                                          start=(m == 0 and sub == 0), stop=(m == 1 and sub == 3), skip_group_check=True),
                             reads=[('pTc', m), ('ones8',)], writes=[PK(6)], inc=(m == 1))
                S.op(DVE, lambda: nc.vector.reciprocal(out=rden[:, 4:8], in_=ps[6][:, 0:32].rearrange("p (a b) -> p a b", b=8)[:, :, 0]), reads=[PK(6)], writes=[('rdenc',)])
                for bk in range(2):
                    S.op(DVE, lambda: nc.vector.tensor_tensor(out=o_tm[:, 2 * bk:2 * bk + 2, hm * 256:(hm + 1) * 256],
                                                              in0=ps[3 + bk][:, :].rearrange("p (a b) -> p a b", a=2),
                                                              in1=rden[:, 4 + 2 * bk:6 + 2 * bk].unsqueeze(2).to_broadcast([128, 2, 256]), op=ALU.mult),
                         reads=[PK(3 + bk), ('rdenc',)], writes=[('o_tm', 2 * bk), ('o_tm', 2 * bk + 1)])
            for sub in range(4):
                t = qj * 4 + sub
                tpb = ps[5][:, :].bitcast(BF16)
                for kc in range(KC):
                    S.op(PE, lambda: nc.tensor.transpose(tpb[:, kc * 128:(kc + 1) * 128], o_tm[:, sub, kc * 128:(kc + 1) * 128], ident_b[:, :]),
                         reads=[('o_tm', sub), ('identb',)], writes=[PK(5)], inc=(kc == KC - 1))
                e = evac_eng()
                S.op(e, copy_op(e, oT_t[:, :, sub * 128:(sub + 1) * 128], tpb[:, 0:1024].rearrange("p (a b) -> p a b", a=KC)),
                     reads=[PK(5)], writes=[('oT', sub)])
                for nh in range(2):
                    wsl, wvw = wos[nh]
                    pb = nxt('psS', 3)
                    for kc in range(KC):
                        S.op(PE, lambda: nc.tensor.matmul(ps[pb][:, :], oT_t[:, kc, sub * 128:(sub + 1) * 128], wvw[:, kc, :],
                                                          start=(kc == 0), stop=(kc == KC - 1)),
                             reads=[('ws', wsl), ('oT', sub)], writes=[PK(pb)], inc=(kc == KC - 1))
                    S.op(DVE, lambda: nc.vector.tensor_tensor(out=H[:, t, nh * 512:(nh + 1) * 512], in0=ps[pb][:, :],
                                                              in1=H[:, t, nh * 512:(nh + 1) * 512], op=ALU.add),
                         reads=[PK(pb), ('H', t)], writes=[('H', t)])
                norm_tile(H[:, t, :], [('H', t)], uT, t * 128, [('uT', t)], t)
        dump('h2', H[:, :, :], [('H', t) for t in range(NT)])
        if stage == 5:
            return finish()
        S.barrier()

        for half in range(2):
            for cg in range(6):
                ncol = 512 if cg < 5 else 256
                sg, wg = load_w(wb_gate, DFF, cg * 512, ncol)
                su, wu = load_w(wb_up, DFF, cg * 512, ncol)
                for ci in range(ncol // 128):
                    c = cg * 4 + ci
                    for tt2 in range(2):
                        tt = half * 2 + tt2
                        pg = nxt('psG', 2)
                        pu = 2 + nxt('psU', 3)
                        rdk = [('uT', 4 * tt + i) for i in range(4)]
                        for kc in range(KC):
                            S.op(PE, lambda: nc.tensor.matmul(ps[pg][:, :], wg[:, kc, ci * 128:(ci + 1) * 128], uT[:, kc, tt * 512:(tt + 1) * 512],
                                                              start=(kc == 0), stop=(kc == KC - 1)),
                                 reads=[('ws', sg)] + rdk, writes=[PK(pg)], inc=(kc == KC - 1))
                        for kc in range(KC):
                            S.op(PE, lambda: nc.tensor.matmul(ps[pu][:, :], wu[:, kc, ci * 128:(ci + 1) * 128], uT[:, kc, tt * 512:(tt + 1) * 512],
                                                              start=(kc == 0), stop=(kc == KC - 1)),
                                 reads=[('ws', su)] + rdk, writes=[PK(pu)], inc=(kc == KC - 1))
                        sl = nxt('a_sb', 2)
                        S.op(ACT, lambda: nc.scalar.copy(out=a_sb[sl][:, 2:514], in_=ps[pg][:, :]), reads=[PK(pg)], writes=[('a_sb', sl)])
                        if tt == 0:
                            S.op(POOL, lambda: nc.gpsimd.memset(a_sb[sl][:, 0:2], 0.0), reads=[('a_sb', sl)], writes=[('a_sb', sl)])
                        elif tt2 == 1:
                            S.op(POOL, lambda: nc.gpsimd.tensor_copy(out=a_sb[sl][:, 0:2], in_=a_sb[1 - sl][:, 512:514]),
                                 reads=[('a_sb', 1 - sl), ('a_sb', sl)], writes=[('a_sb', sl)])
                        else:
                            S.op(POOL, lambda: nc.gpsimd.tensor_copy(out=a_sb[sl][:, 0:2], in_=halo[:, c, :]),
                                 reads=[('halo',), ('a_sb', sl)], writes=[('a_sb', sl)])
                        if tt == 1:
                            S.op(POOL, lambda: nc.gpsimd.tensor_copy(out=halo[:, c, :], in_=a_sb[sl][:, 512:514]),
                                 reads=[('a_sb', sl)], writes=[('halo',)])
                        S.op(ACT, lambda: nc.scalar.activation(out=t1[sl][:, :], in_=ps[pg][:, :], func=AF.Identity,
                                                               scale=convc[:, c, 2:3], bias=convc[:, c, 3:4]),
                             reads=[PK(pg), ('convc',)], writes=[('t1', sl)])
                        S.op(DVE, lambda: nc.vector.scalar_tensor_tensor(out=t1[sl][:, :], in0=a_sb[sl][:, 1:513], scalar=convc[:, c, 1:2],
                                                                         in1=t1[sl][:, :], op0=ALU.mult, op1=ALU.add),
                             reads=[('a_sb', sl), ('t1', sl), ('convc',)], writes=[('t1', sl)])
                        S.op(DVE, lambda: nc.vector.scalar_tensor_tensor(out=t1[sl][:, :], in0=a_sb[sl][:, 0:512], scalar=convc[:, c, 0:1],
                                                                         in1=t1[sl][:, :], op0=ALU.mult, op1=ALU.add),
                             reads=[('a_sb', sl), ('t1', sl), ('convc',)], writes=[('t1', sl)])
                        S.op(ACT, lambda: nc.scalar.activation(out=t1[sl][:, :], in_=t1[sl][:, :], func=AF.Silu),
                             reads=[('t1', sl)], writes=[('t1', sl)])
                        S.op(DVE, lambda: nc.vector.tensor_tensor(out=gT[:, c, tt2 * 512:(tt2 + 1) * 512], in0=t1[sl][:, :], in1=ps[pu][:, :], op=ALU.mult),
                             reads=[('t1', sl), PK(pu)], writes=[('gT', c, tt2)])
            for nh in range(2):
                wds = []
                for j in range(3):
                    nkc = 8 if j < 2 else 6
                    wds.append(load_w(wb_down, D, nh * 512, 512, row0=j * 1024, nkc=nkc))
                for tl in range(8):
                    t = half * 8 + tl
                    pb = 5 + nxt('psD', 3)
                    for kc in range(NFC):
                        wsl, wvw = wds[kc // 8]
                        S.op(PE, lambda: nc.tensor.matmul(ps[pb][:, :], gT[:, kc, tl * 128:(tl + 1) * 128], wvw[:, kc % 8, :],
                                                          start=(kc == 0), stop=(kc == NFC - 1)),
                             reads=[('ws', wsl), ('gT', kc, tl // 4)], writes=[PK(pb)], inc=(kc == NFC - 1))
                    S.op(DVE, lambda: nc.vector.tensor_tensor(out=H[:, t, nh * 512:(nh + 1) * 512], in0=ps[pb][:, :],
                                                              in1=H[:, t, nh * 512:(nh + 1) * 512], op=ALU.add),
                         reads=[PK(pb), ('H', t)], writes=[('H', t)])
                    if nh == 1:
                        S.op(ACT, lambda: nc.scalar.activation(out=junk[:, :], in_=H[:, t, :], func=AF.Square, accum_out=stat[:, 0, t:t + 1]),
                             reads=[('H', t)], writes=[('junk',), ('ss', t)])
                        rstd_from_ss(stat[:, 0, t:t + 1], stat[:, 1, t:t + 1], stat[:, 2, t:t + 1], D, [('ss', t)], ('rstd', t))
                        xs = nxt('xt', 2)
                        S.op(DVE, lambda: nc.vector.scalar_tensor_tensor(out=xt[xs][:, :], in0=H[:, t, :], scalar=stat[:, 2, t:t + 1], in1=gfin[:, :],
                                                                         op0=ALU.mult, op1=ALU.mult),
                             reads=[('H', t), ('rstd', t), ('gfin',)], writes=[('xt', xs)])
                        out_toks.append(S.dma('sp', out=out_d[b, t * 128:(t + 1) * 128, :], in_=xt[xs][:, :], reads=[('xt', xs)]))

    return finish()


_CACHE = {}


def kernel(**inputs):
    inp = {k: np.asarray(v) for k, v in inputs.items()}
    if 'nc' not in _CACHE:
        _CACHE['nc'] = build_program(NB_CORE)[0]
    nc = _CACHE['nc']
    consts = host_consts()
    shared = {
        "w_in": inp["w_in"].reshape(D, 3072), "w_out": inp["w_out"].reshape(D, D),
        "w_q_mem": inp["w_q_mem"].reshape(D, D), "w_kv_mem": inp["w_kv_mem"].reshape(D, 2 * D),
        "w_o_mem": inp["w_o_mem"].reshape(D, D), "w_gate": inp["w_gate"].reshape(D, DFF),
        "w_up": inp["w_up"].reshape(D, DFF), "w_down": inp["w_down"].reshape(DFF, D),
        "g_mix": inp["g_mix"].reshape(D), "g_cross": inp["g_cross"].reshape(D), "g_mem": inp["g_mem"].reshape(D),
        "g_ffn": inp["g_ffn"].reshape(D), "g_out_dil": inp["g_out_dil"].reshape(512),
        "g_out_moba": inp["g_out_moba"].reshape(512), "g_final": inp["g_final"].reshape(D),
        "rel_bias": inp["rel_bias"].reshape(16, 32), "conv_w": inp["conv_w"].reshape(3, DFF),
        "conv_b": inp["conv_b"].reshape(DFF),
    }
    shared = {k: np.ascontiguousarray(v, dtype=np.float32) for k, v in shared.items()}
    shared.update(consts)
    x = np.ascontiguousarray(inp["x"], dtype=np.float32)
    mem = np.ascontiguousarray(inp["mem"], dtype=np.float32)
    in_maps = []
    for i in range(NCORES):
        m = dict(shared)
        m["x"] = x[i * NB_CORE:(i + 1) * NB_CORE]
        m["mem"] = mem[i * NB_CORE:(i + 1) * NB_CORE]
        in_maps.append(m)
    res = run_bass_kernel_spmd(nc, in_maps, core_ids=list(range(NCORES)))
    return np.concatenate([np.asarray(r["out"]) for r in res.results], axis=0).astype(np.float32)
```

```python
import math
import numpy as np
import concourse.bass as bass
import concourse.mybir as mybir
from concourse.bass_utils import run_bass_kernel_spmd

F32 = mybir.dt.float32
BF16 = mybir.dt.bfloat16
AF = mybir.ActivationFunctionType
ALU = mybir.AluOpType
AX = mybir.AxisListType

S_LEN = 2048
D = 1024
NT = 16
KC = 8
DFF = 2816
NFC = 22
NMEM = 256
EW = 2176
PITCH = 2304
EPS = 1e-6
NCORES = 8
NB_CORE = 4
SB_BASE = 16640


class Sch:
    CH = 16000
    NDS = 24

    def __init__(self, nc):
        self.nc = nc
        self.E = {'pe': nc.tensor, 'act': nc.scalar, 'dve': nc.vector, 'pool': nc.gpsimd, 'sp': nc.sync}
        self.cnt = {e: 0 for e in self.E}
        self.sems = {e: [] for e in self.E}
        self.seen = {e: {} for e in self.E}
        self.dsem = [nc.alloc_semaphore("dsem%d" % i) for i in range(self.NDS)]
        self.dval = [0] * self.NDS
        self.dnext = {'sp': 0, 'pool': 0}
        self.lastw = {}
        self.rd_e = {}
        self.rd_d = {}
        self.nwaits = 0

    def _sem(self, e, k):
        g = (k - 1) // self.CH
        while len(self.sems[e]) <= g:
            self.sems[e].append(self.nc.alloc_semaphore("s_%s_%d" % (e, len(self.sems[e]))))
        return self.sems[e][g], (k - 1) % self.CH + 1

    def wait(self, e, tok):
        if tok is None:
            return
        if tok[0] == 'e':
            _, e2, k = tok
            if e2 == e and e in ('pe', 'sp'):
                return
            assert k <= self.cnt[e2], ("wait on un-emitted inc", e, tok, self.cnt[e2])
            if self.seen[e].get(('e', e2), 0) >= k:
                return
            sem, v = self._sem(e2, k)
            self.E[e].wait_ge(sem, v)
            self.seen[e][('e', e2)] = k
        else:
            _, s, v = tok
            if self.seen[e].get(('d', s), 0) >= v:
                return
            self.E[e].wait_ge(self.dsem[s], v)
            self.seen[e][('d', s)] = v
        self.nwaits += 1

    def deps(self, e, reads, writes):
        for key in reads:
            self.wait(e, self.lastw.get(key))
        for key in writes:
            self.wait(e, self.lastw.get(key))
            for e2, k in self.rd_e.get(key, {}).items():
                self.wait(e, ('e', e2, k))
            for t in self.rd_d.get(key, ()):
                self.wait(e, t)

    def commit(self, tok, reads, writes):
        for key in reads:
            if tok[0] == 'e':
                d = self.rd_e.setdefault(key, {})
                d[tok[1]] = max(d.get(tok[1], 0), tok[2])
            else:
                self.rd_d.setdefault(key, []).append(tok)
        for key in writes:
            self.lastw[key] = tok
            self.rd_e[key] = {}
            self.rd_d[key] = []

    def op(self, e, fn, reads=(), writes=(), inc=True):
        self.deps(e, reads, writes)
        ins = fn()
        if inc:
            self.cnt[e] += 1
            sem, v = self._sem(e, self.cnt[e])
            ins.then_inc(sem, 1)
            tok = ('e', e, self.cnt[e])
        else:
            tok = ('e', e, self.cnt[e] + 1)
        self.commit(tok, reads, writes)
        return tok

    def dma(self, q, out, in_, reads=(), writes=()):
        half = self.NDS // 2
        i = self.dnext[q]
        self.dnext[q] = (i + 1) % half
        s = i + (half if q == 'pool' else 0)
        if self.dval[s] > 0:
            self.wait(q, ('d', s, self.dval[s]))
        self.deps(q, reads, writes)
        ins = self.E[q].dma_start(out=out, in_=in_)
        self.dval[s] += 16
        ins.then_inc(self.dsem[s], 16)
        tok = ('d', s, self.dval[s])
        self.commit(tok, reads, writes)
        return tok

    def barrier(self, engines=('pe', 'act', 'dve', 'pool')):
        for e2 in engines:
            if e2 != 'dve' and self.cnt[e2] > 0:
                self.wait('dve', ('e', e2, self.cnt[e2]))
        tok = self.op('dve', lambda: self.nc.vector.memset(self.bar[:, :], 0.0), writes=[('bar',)])
        for e in engines:
            if e != 'dve':
                self.wait(e, tok)


def _bucket_table():
    n = np.arange(0, 2048, dtype=np.int64)
    nf = np.maximum(n, 1).astype(np.float32)
    large = 16 + (np.log(nf / np.float32(16)) / np.float32(math.log(128.0)) * np.float32(16)).astype(np.int32)
    large = np.minimum(large, 31)
    return np.where(n < 16, n, large).astype(np.int64)


def host_consts():
    bk = _bucket_table()
    onehot = np.zeros((32, EW), np.float32)
    d = np.arange(2048)
    onehot[bk, d + 128] = 1.0
    mult = np.zeros((2, EW), np.float32)
    mA = (d <= 128).astype(np.float32) + ((d % 4 == 0) & (d <= 512)) + ((d % 16 == 0) & (d <= 2048))
    mult[0, 128:] = mA
    mult[1, 128:] = 1.0
    multr = np.ascontiguousarray(np.broadcast_to(mult[None], (128, 2, EW))).astype(np.float32)
    mob = np.zeros((3, 16, 8), np.float32)
    for qt in range(16):
        own = qt // 2
        for n in range(8):
            mob[0, qt, n] = 0.0 if n < own else -1e30
            mob[1, qt, n] = 1.0 if n < own else 0.0
            mob[2, qt, n] = 1.0 if n == own else 0.0
    mobr = np.ascontiguousarray(np.broadcast_to(mob.reshape(1, -1), (128, 384))).astype(np.float32)
    selhot = np.zeros((128, 8, 128), np.float32)
    for base in (0, 32, 64):
        for n in range(8):
            selhot[base + n, n, :] = 1.0
    return {"c_onehot": onehot, "c_mult": multr.reshape(128, 2 * EW), "c_moba": mobr,
            "c_selhot": selhot.reshape(128, 1024)}


def build_program(NB=NB_CORE, dbg=(), stage=99):
    nc = bass.Bass("TRN2", target_bir_lowering=False)
    S = Sch(nc)
    PE, ACT, DVE, POOL = 'pe', 'act', 'dve', 'pool'

    def din(name, shape, dt=F32):
        return nc.dram_tensor(name, list(shape), dt, kind="ExternalInput")

    x_d = din("x", [NB, S_LEN, D])
    mem_d = din("mem", [NB, NMEM, D])
    w_in_d = din("w_in", [D, 3072])
    w_out_d = din("w_out", [D, D])
    w_q_d = din("w_q_mem", [D, D])
    w_kv_d = din("w_kv_mem", [D, 2 * D])
    w_o_d = din("w_o_mem", [D, D])
    w_gate_d = din("w_gate", [D, DFF])
    w_up_d = din("w_up", [D, DFF])
    w_down_d = din("w_down", [DFF, D])
    g_mix_d = din("g_mix", [D])
    g_cross_d = din("g_cross", [D])
    g_mem_d = din("g_mem", [D])
    g_ffn_d = din("g_ffn", [D])
    g_od_d = din("g_out_dil", [512])
    g_om_d = din("g_out_moba", [512])
    g_fin_d = din("g_final", [D])
    relb_d = din("rel_bias", [16, 32])
    convw_d = din("conv_w", [3, DFF])
    convb_d = din("conv_b", [DFF])
    c_onehot_d = din("c_onehot", [32, EW])
    c_mult_d = din("c_mult", [128, 2 * EW])
    c_moba_d = din("c_moba", [128, 384])
    c_selhot_d = din("c_selhot", [128, 1024])
    out_d = nc.dram_tensor("out", [NB, S_LEN, D], F32, kind="ExternalOutput")
    dbg_t = {}
    for nm in dbg:
        dbg_t[nm] = nc.dram_tensor("dbg_" + nm, [S_LEN, D], F32, kind="ExternalOutput")

    wb_in = nc.dram_tensor("wb_in", [D, 3072], BF16)
    wb_out = nc.dram_tensor("wb_out", [D, D], BF16)
    wb_q = nc.dram_tensor("wb_q", [D, D], BF16)
    wb_kv = nc.dram_tensor("wb_kv", [D, 2 * D], BF16)
    wb_o = nc.dram_tensor("wb_o", [D, D], BF16)
    wb_gate = nc.dram_tensor("wb_gate", [D, DFF], BF16)
    wb_up = nc.dram_tensor("wb_up", [D, DFF], BF16)
    wb_down = nc.dram_tensor("wb_down", [DFF, D], BF16)
    tz_scr = nc.dram_tensor("tz_scr", [16 * 128 * PITCH], BF16)

    def sb(name, shape, dt, off):
        return nc.alloc_sbuf_tensor_at(name, list(shape), dt, offset=SB_BASE + off)

    o = 0
    ident_b = sb("ident_b", [128, 128], BF16, o); o += 256
    ident_f = sb("ident_f", [128, 128], F32, o); o += 512
    selhot = sb("selhot", [128, 8, 128], BF16, o); o += 2048
    mobac = sb("mobac", [128, 3, 16, 8], F32, o); o += 1536
    gfin = sb("gfin", [128, D], F32, o); o += 4096
    convc = sb("convc", [128, NFC, 4], F32, o); o += 352
    gsb = sb("gsb", [128, 5, 8], F32, o); o += 160
    stat = sb("stat", [128, 4, 16], F32, o); o += 256
    staty = sb("staty", [128, 4, 4], F32, o); o += 64
    rden = sb("rden", [128, 8], F32, o); o += 32
    ones8 = sb("ones8", [128, 8], BF16, o); o += 32
    S.bar = sb("bar", [128, 8], F32, o); o += 32
    assert o <= 10240
    WS0 = 10240
    ws = [sb("ws%d" % i, [128, 4096], BF16, WS0 + 8192 * i) for i in range(4)]
    XT0 = WS0 + 32768
    xt = [sb("xt%d" % i, [128, D], F32, XT0 + 4096 * i) for i in range(2)]
    SC0 = XT0 + 8192
    junk = sb("junk", [128, D], BF16, SC0)
    u_tm = [sb("u_tm%d" % i, [128, D], BF16, SC0 + 2048 + 2048 * i) for i in range(2)]
    ybf = [sb("ybf%d" % i, [128, 512], BF16, SC0 + 6144 + 1024 * i) for i in range(2)]
    A2_0 = SC0 + 8192
    uT = sb("uT", [128, KC, S_LEN], BF16, A2_0)
    A1_0 = A2_0 + 32768
    A3_0 = A1_0 + 87744
    assert A3_0 + 32768 + SB_BASE <= 229368
    H = sb("H", [128, NT, D], F32, A1_0)
    o = A1_0
    qkT = sb("qkT", [128, 8, S_LEN], BF16, o); o += 32768
    V = sb("V", [128, NT, 8, 65], BF16, o); o += 16640
    tzb = [sb("tzb%d" % i, [128, 2048], BF16, o + 4096 * i) for i in range(3)]; o += 12288
    ytm = sb("ytm", [128, 4, 512], F32, o); o += 8192
    pexp = [sb("pexp%d" % i, [128, 512], BF16, o + 1024 * i) for i in range(4)]; o += 4096
    pT = [sb("pT%d" % i, [128, 512], BF16, o + 1024 * i) for i in range(4)]; o += 4096
    selT = sb("selT", [128, 4, 512], BF16, o); o += 4096
    selsrc = sb("selsrc", [128, 4, 128], BF16, o); o += 2048
    gm = sb("gm", [128, 32, 8], F32, o); o += 1024
    gm2 = sb("gm2", [128, 32, 8], F32, o); o += 1024
    gsel = sb("gsel", [128, 32, 8], F32, o); o += 1024
    gmx = sb("gmx", [128, 32], F32, o); o += 128
    ksum = sb("ksum", [128, 4, 8], F32, o); o += 128
    kmT = sb("kmT", [128, 4, 8], BF16, o); o += 64
    kmTp = sb("kmTp", [128, 8, 8], BF16, o); o += 128
    assert o <= A3_0, o
    yT = sb("yT", [128, 8, S_LEN], BF16, A3_0)
    o = A3_0
    qmT_t = sb("qmT_t", [128, 8, 512], BF16, o); o += 8192
    o_tm = sb("o_tm", [128, 4, D], BF16, o); o += 8192
    oT_t = sb("oT_t", [128, 8, 512], BF16, o); o += 8192
    pTc = [sb("pTc%d" % i, [128, 512], BF16, o + 1024 * i) for i in range(2)]; o += 2048
    TL0 = A1_0 + 65536
    o = TL0
    kmTc = sb("kmTc", [128, 8, NMEM], BF16, o); o += 4096
    vm = sb("vm", [128, 2, 4, 257], BF16, o); o += 4112 + 16
    mnT = sb("mnT", [128, 8, NMEM], BF16, o); o += 4096
    assert o <= A3_0
    gT = sb("gT", [128, NFC, 1024], BF16, TL0); o = TL0 + 45056
    a_sb = [sb("a_sb%d" % i, [128, 514], F32, o + 2080 * i) for i in range(2)]; o += 4160
    t1 = [sb("t1_%d" % i, [128, 512], F32, o + 2048 * i) for i in range(2)]; o += 4096
    halo = sb("halo", [128, NFC, 2], F32, o); o += 176
    assert o <= A3_0 + 32768, o
    stf = [sb("stf%d" % i, [128, 3072], F32, A1_0 + 12288 * i) for i in range(3)]
    stb = [sb("stb%d" % i, [128, 3072], BF16, A1_0 + 36864 + 6144 * i) for i in range(3)]
    mult_sb = sb("mult_sb", [128, 2, EW], F32, A3_0)
    onehot_sb = sb("onehot_sb", [32, EW], F32, A3_0 + 17408)
    rbT = sb("rbT", [32, 16], F32, A3_0 + 17408 + 8704)
    lh = [sb("lh%d" % i, [32, 128], F32, A3_0 + 17408 + 8704 + 64 + 512 * i) for i in range(2)]
    erow = sb("erow", [128, EW], F32, A2_0)
    e_bf = [sb("e_bf%d" % i, [128, EW], BF16, A2_0 + 8704 + 4352 * i) for i in range(2)]

    ps = [nc.alloc_psum_tensor("ps%d" % i, [128, 512], F32) for i in range(8)]

    def PK(i):
        return ('ps', i)

    rot = {}

    def nxt(name, n):
        v = rot.get(name, 0)
        rot[name] = (v + 1) % n
        return v

    def evac_eng():
        return (ACT, DVE)[nxt('evac', 2)]

    def copy_op(e, out, in_):
        if e == ACT:
            return lambda: nc.scalar.copy(out=out, in_=in_)
        if e == DVE:
            return lambda: nc.vector.tensor_copy(out=out, in_=in_)
        return lambda: nc.gpsimd.tensor_copy(out=out, in_=in_)

    def dump(nm, ap, reads):
        if nm not in dbg_t:
            return
        tok = S.dma('sp', out=dbg_t[nm][:, :].rearrange("(t p) d -> p t d", p=128), in_=ap, reads=reads)
        for e in ('pe', 'act', 'dve', 'pool'):
            S.wait(e, tok)

    out_toks = []

    def finish():
        for tok in out_toks:
            S.wait('sp', tok)
        for s_ in range(S.NDS):
            if S.dval[s_] > 0:
                S.wait('sp', ('d', s_, S.dval[s_]))
        for e in ('pe', 'act', 'dve', 'pool'):
            if S.cnt[e] > 0:
                S.wait('sp', ('e', e, S.cnt[e]))
        return nc, S

    with nc.allow_non_contiguous_dma(reason="tiny one-time parameter loads"):
        for i, gt in enumerate([g_mix_d, g_cross_d, g_mem_d, g_ffn_d]):
            S.dma('sp', out=gsb[:, i, :], in_=bass.AP(gt, 0, [[1, 128], [128, 8]]), writes=[('gsb',)])
        S.dma('sp', out=gsb[:, 4, 0:4], in_=bass.AP(g_od_d, 0, [[1, 128], [128, 4]]), writes=[('gsb',)])
        S.dma('sp', out=gsb[:, 4, 4:8], in_=bass.AP(g_om_d, 0, [[1, 128], [128, 4]]), writes=[('gsb',)])
        for j in range(3):
            S.dma('sp', out=convc[:, :, j], in_=bass.AP(convw_d, j * DFF, [[1, 128], [128, NFC]]), writes=[('convc',)])
        S.dma('sp', out=convc[:, :, 3], in_=bass.AP(convb_d, 0, [[1, 128], [128, NFC]]), writes=[('convc',)])
        S.dma('sp', out=rbT[:, :], in_=bass.AP(relb_d, 0, [[1, 32], [32, 16]]), writes=[('rbT',)])
    S.dma('sp', out=gfin[:, :], in_=bass.AP(g_fin_d, 0, [[0, 128], [1, D]]), writes=[('gfin',)])
    S.dma('sp', out=mobac[:, :, :, :].rearrange("p a b c -> p (a b c)"), in_=c_moba_d[:, :], writes=[('mobac',)])
    S.dma('sp', out=mult_sb[:, :, :].rearrange("p a b -> p (a b)"), in_=c_mult_d[:, :], writes=[('mult',)])
    S.dma('sp', out=onehot_sb[:, :], in_=c_onehot_d[:, :], writes=[('onehot',)])
    S.dma('sp', out=stf[0][:, 0:1024], in_=c_selhot_d[:, :], writes=[('stf', 0)])
    S.op(DVE, lambda: nc.vector.tensor_copy(out=selhot[:, :, :].rearrange("p a b -> p (a b)"), in_=stf[0][:, 0:1024]),
         reads=[('stf', 0)], writes=[('selhot',)])
    S.op(POOL, lambda: nc.gpsimd.memset(ident_b[:, :], 0.0), writes=[('identb',)])
    S.op(POOL, lambda: nc.gpsimd.affine_select(out=ident_b[:, :], in_=ident_b[:, :], pattern=[[-1, 128]],
                                              compare_op=ALU.not_equal, fill=1.0, base=0, channel_multiplier=1),
         reads=[('identb',)], writes=[('identb',)])
    S.op(POOL, lambda: nc.gpsimd.memset(ones8[:, :], 1.0), writes=[('ones8',)])
    S.op(POOL, lambda: nc.gpsimd.memset(ident_f[:, :], 0.0), writes=[('identf',)])
    S.op(POOL, lambda: nc.gpsimd.affine_select(out=ident_f[:, :], in_=ident_f[:, :], pattern=[[-1, 128]],
                                              compare_op=ALU.not_equal, fill=1.0, base=0, channel_multiplier=1),
         reads=[('identf',)], writes=[('identf',)])

    wstep = [0]

    def conv_w(src, K, N, dst, gi):
        for kc in range(K // 128):
            st = wstep[0]
            wstep[0] += 1
            sl = st % 3
            S.dma('sp', out=stf[sl][:, 0:N], in_=src[kc * 128:(kc + 1) * 128, :], writes=[('stf', sl)])
            e = (DVE, ACT)[st % 2]
            o_ap = stb[sl][:, 0:N]
            i_ap = stf[sl][:, 0:N]
            if gi is None:
                fn = copy_op(e, o_ap, i_ap)
                rd = [('stf', sl)]
            else:
                g_ap = gsb[:, gi, kc:kc + 1]
                rd = [('stf', sl), ('gsb',)]
                if e == ACT:
                    fn = lambda o_ap=o_ap, i_ap=i_ap, g_ap=g_ap: nc.scalar.activation(out=o_ap, in_=i_ap, func=AF.Copy, scale=g_ap)
                elif e == DVE:
                    fn = lambda o_ap=o_ap, i_ap=i_ap, g_ap=g_ap: nc.vector.tensor_scalar(out=o_ap, in0=i_ap, scalar1=g_ap, scalar2=None, op0=ALU.mult)
                else:
                    fn = lambda o_ap=o_ap, i_ap=i_ap, g_ap=g_ap: nc.gpsimd.tensor_scalar(out=o_ap, in0=i_ap, scalar1=g_ap, scalar2=None, op0=ALU.mult)
            S.op(e, fn, reads=rd, writes=[('stb', sl)])
            S.dma('pool', out=dst[kc * 128:(kc + 1) * 128, :], in_=stb[sl][:, 0:N], reads=[('stb', sl)], writes=[('wscr',)])

    conv_w(w_in_d, D, 3072, wb_in, 0)
    conv_w(w_out_d, D, D, wb_out, 4)
    conv_w(w_q_d, D, D, wb_q, 1)
    conv_w(w_kv_d, D, 2 * D, wb_kv, 2)
    conv_w(w_o_d, D, D, wb_o, None)
    conv_w(w_gate_d, D, DFF, wb_gate, 3)
    conv_w(w_up_d, D, DFF, wb_up, 3)
    conv_w(w_down_d, DFF, D, wb_down, None)

    for h in range(16):
        grp = 0 if h < 8 else 1
        sl = h % 2
        S.op(DVE, lambda: nc.vector.tensor_copy(out=lh[sl][:, :], in_=rbT[:, h:h + 1].to_broadcast([32, 128])),
             reads=[('rbT',)], writes=[('lh', sl)])
        for j in range(5):
            c0 = j * 512
            w = min(512, EW - c0)
            S.op(PE, lambda: nc.tensor.matmul(ps[j][:, 0:w], lh[sl][:, :], onehot_sb[:, c0:c0 + w], start=True, stop=True),
                 reads=[('lh', sl), ('onehot',)], writes=[PK(j)])
            S.op(ACT, lambda: nc.scalar.activation(out=erow[:, c0:c0 + w], in_=ps[j][:, 0:w], func=AF.Exp),
                 reads=[PK(j)], writes=[('erow', j)])
            S.op(DVE, lambda: nc.vector.tensor_tensor(out=e_bf[sl][:, c0:c0 + w], in0=erow[:, c0:c0 + w],
                                                      in1=mult_sb[:, grp, c0:c0 + w], op=ALU.mult),
                 reads=[('erow', j), ('mult',)], writes=[('e_bf', sl)])
        S.dma('pool', out=bass.AP(tz_scr, h * 128 * PITCH, [[PITCH + 1, 128], [1, EW]]), in_=e_bf[sl][:, :],
              reads=[('e_bf', sl)], writes=[('tzscr',)])

    for e in ('sp', 'pool', 'pe', 'act', 'dve'):
        S.wait(e, S.lastw.get(('wscr',)))
        S.wait(e, S.lastw.get(('tzscr',)))
        for s_ in range(S.NDS):
            if S.dval[s_] > 0:
                S.wait(e, ('d', s_, S.dval[s_]))
    S.barrier()

    def rstd_from_ss(ss_ap, v_ap, r_ap, n_feat, keys_in, key_out):
        S.op(DVE, lambda: nc.vector.tensor_scalar(out=v_ap, in0=ss_ap, scalar1=1.0 / n_feat, scalar2=EPS,
                                                  op0=ALU.mult, op1=ALU.add), reads=keys_in, writes=[key_out + ('v',)])
        S.op(ACT, lambda: nc.scalar.activation(out=v_ap, in_=v_ap, func=AF.Ln), reads=[key_out + ('v',)], writes=[key_out + ('v',)])
        S.op(ACT, lambda: nc.scalar.activation(out=r_ap, in_=v_ap, func=AF.Exp, scale=-0.5), reads=[key_out + ('v',)], writes=[key_out])

    def norm_tile(src_ap, src_keys, dstT, col0, dst_keys, idx):
        S.op(ACT, lambda: nc.scalar.activation(out=junk[:, :], in_=src_ap, func=AF.Square, accum_out=stat[:, 0, idx:idx + 1]),
             reads=src_keys, writes=[('junk',), ('ss', idx)])
        rstd_from_ss(stat[:, 0, idx:idx + 1], stat[:, 1, idx:idx + 1], stat[:, 2, idx:idx + 1], D, [('ss', idx)], ('rstd', idx))
        us = nxt('u_tm', 2)
        S.op(POOL, lambda: nc.gpsimd.tensor_scalar(out=u_tm[us][:, :], in0=src_ap, scalar1=stat[:, 2, idx:idx + 1], scalar2=None, op0=ALU.mult),
             reads=list(src_keys) + [('rstd', idx)], writes=[('u_tm', us)])
        tpb = ps[5][:, :].bitcast(BF16)
        for kc in range(KC):
            S.op(PE, lambda: nc.tensor.transpose(tpb[:, kc * 128:(kc + 1) * 128], u_tm[us][:, kc * 128:(kc + 1) * 128], ident_b[:, :]),
                 reads=[('u_tm', us), ('identb',)], writes=[PK(5)], inc=(kc == KC - 1))
        e = evac_eng()
        S.op(e, copy_op(e, dstT[:, :, col0:col0 + 128], tpb[:, 0:1024].rearrange("p (a b) -> p a b", a=KC)),
             reads=[PK(5)], writes=dst_keys)

    def load_w(scr, ncols_total, col0, ncols, row0=0, nkc=KC):
        s = nxt('ws', 4)
        view = ws[s][:, 0:nkc * ncols].rearrange("p (a b) -> p a b", a=nkc)
        S.dma('sp', out=view, in_=bass.AP(scr, row0 * ncols_total + col0, [[ncols_total, 128], [128 * ncols_total, nkc], [1, ncols]]),
              writes=[('ws', s)])
        return s, view

    if stage == 0:
        return finish()
    for b in range(NB):
        S.barrier()
        for t in range(NT):
            xs = nxt('xt', 2)
            S.dma('pool', out=xt[xs][:, :], in_=x_d[b, t * 128:(t + 1) * 128, :], writes=[('xt', xs)])
            norm_tile(xt[xs][:, :], [('xt', xs)], uT, t * 128, [('uT', t)], t)

        if stage == 1:
            dump('h1', uT[:, :, :].bitcast(F32).rearrange("p a (b c) -> p (a b) c", c=1024)[:, :, :] if False else H[:, :, :], [])
            return finish()
        S.op(POOL, lambda: nc.gpsimd.memset(V[:, :, :, 64:65], 1.0), writes=[('Vones',)])
        S.op(POOL, lambda: nc.gpsimd.memset(selsrc[:, :, :], 0.0), writes=[('selsrc',)])

        for g in range(2):
            cb = g * 1536
            sq, wq = load_w(wb_in, 3072, cb, 512)
            sk, wk = load_w(wb_in, 3072, cb + 512, 512)
            sv, wv = load_w(wb_in, 3072, cb + 1024, 512)
            for c in range(8):
                wsl, wvw = (sq, wq) if c < 4 else (sk, wk)
                for tt in range(4):
                    pb = nxt('psS', 3)
                    for kc in range(KC):
                        S.op(PE, lambda: nc.tensor.matmul(ps[pb][:, :], wvw[:, kc, (c % 4) * 128:(c % 4 + 1) * 128],
                                                          uT[:, kc, tt * 512:(tt + 1) * 512], start=(kc == 0), stop=(kc == KC - 1)),
                             reads=[('ws', wsl)] + [('uT', 4 * tt + i) for i in range(4)], writes=[PK(pb)], inc=(kc == KC - 1))
                    e = evac_eng()
                    S.op(e, copy_op(e, qkT[:, c, tt * 512:(tt + 1) * 512], ps[pb][:, :]), reads=[PK(pb)], writes=[('qk', c, tt)])
                    if g == 1 and c >= 4:
                        S.op(DVE, lambda: nc.vector.tensor_reduce(out=ksum[:, c - 4, tt * 2:(tt + 1) * 2],
                                                                  in_=qkT[:, c, tt * 512:(tt + 1) * 512].rearrange("p (a b) -> p a b", a=2), axis=AX.X, op=ALU.add),
                             reads=[('qk', c, tt)], writes=[('ksum',)])
            for t in range(NT):
                pb = nxt('psS', 3)
                for kc in range(KC):
                    S.op(PE, lambda: nc.tensor.matmul(ps[pb][:, :], uT[:, kc, t * 128:(t + 1) * 128], wv[:, kc, :],
                                                      start=(kc == 0), stop=(kc == KC - 1)),
                         reads=[('ws', sv), ('uT', t)], writes=[PK(pb)], inc=(kc == KC - 1))
                e = evac_eng()
                S.op(e, copy_op(e, V[:, t, :, 0:64], ps[pb][:, :].rearrange("p (a b) -> p a b", a=8)),
                     reads=[PK(pb), ('Vones',)], writes=[('V', t)])
            if g == 1:
                S.op(DVE, lambda: nc.vector.memset(kmTp[:, :, :], 0.0), writes=[('kmT',)])
                for hp_ in range(2):
                    S.op(DVE, lambda: nc.vector.tensor_scalar(
                        out=kmTp[hp_ * 64:(hp_ + 1) * 64, :, :].rearrange("p (a two) n -> p a two n", two=2)[:, :, hp_, :],
                        in0=ksum[hp_ * 64:(hp_ + 1) * 64, :, :], scalar1=1.0 / 256.0, scalar2=None, op0=ALU.mult),
                         reads=[('ksum',), ('kmT',)], writes=[('kmT',)])

            if stage == 2:
                return finish()
            if stage == 26 and g == 1:
                return finish()
            for qj in range(4):
                q0 = qj * 512
                if g == 1:
                    for sub in range(4):
                        qt = qj * 4 + sub
                        for h in range(8):
                            hp = h % 2
                            S.op(PE, lambda: nc.tensor.matmul(ps[6][:, sub * 64 + h * 8: sub * 64 + h * 8 + 8],
                                                              qkT[:, h // 2, qt * 128:(qt + 1) * 128],
                                                              kmTp[:, h, :], start=True, stop=True),
                                 reads=[('qk', h // 2, qj), ('kmT',)], writes=[PK(6)], inc=(sub == 3 and h == 7))
                    g4 = lambda t_: t_[:, :, :].rearrange("p (s h) n -> p s h n", s=4)
                    pmask = lambda k_: mobac[:, k_, qj * 4:(qj + 1) * 4, :].unsqueeze(2).to_broadcast([128, 4, 8, 8])
                    S.op(DVE, lambda: nc.vector.tensor_tensor(out=g4(gm), in0=ps[6][:, 0:256].rearrange("p (s h n) -> p s h n", s=4, h=8),
                                                              in1=pmask(0), op=ALU.add), reads=[PK(6), ('mobac',)], writes=[('gm',)])
                    src = gm
                    for it in range(3):
                        S.op(DVE, lambda: nc.vector.tensor_reduce(out=gmx[:, :], in_=src[:, :, :], axis=AX.X, op=ALU.max),
                             reads=[('gm',), ('gm2',)], writes=[('gmx',)])
                        if it < 2:
                            S.op(DVE, lambda: nc.vector.tensor_tensor(out=gsel[:, :, :], in0=src[:, :, :],
                                                                      in1=gmx[:, :].unsqueeze(2).to_broadcast([128, 32, 8]), op=ALU.is_ge),
                                 reads=[('gm',), ('gm2',), ('gmx',)], writes=[('gsel',)])
                            S.op(DVE, lambda: nc.vector.scalar_tensor_tensor(out=gm2[:, :, :], in0=gsel[:, :, :], scalar=-1e30, in1=src[:, :, :],
                                                                             op0=ALU.mult, op1=ALU.add),
                                 reads=[('gsel',), ('gm',), ('gm2',)], writes=[('gm2',)])
                            src = gm2
                    S.op(DVE, lambda: nc.vector.tensor_tensor(out=gsel[:, :, :], in0=gm[:, :, :],
                                                              in1=gmx[:, :].unsqueeze(2).to_broadcast([128, 32, 8]), op=ALU.is_ge),
                         reads=[('gm',), ('gmx',)], writes=[('gsel',)])
                    S.op(DVE, lambda: nc.vector.tensor_tensor(out=g4(gsel), in0=g4(gsel), in1=pmask(1), op=ALU.mult),
                         reads=[('gsel',), ('mobac',)], writes=[('gsel',)])
                    S.op(DVE, lambda: nc.vector.tensor_tensor(out=g4(gsel), in0=g4(gsel), in1=pmask(2), op=ALU.add),
                         reads=[('gsel',), ('mobac',)], writes=[('gsel',)])
                    for sub in range(4):
                        for gi in range(4):
                            S.op(DVE, lambda: nc.vector.tensor_scalar(
                                out=selsrc[:, gi, :].rearrange("p (h c) -> p h c", c=64)[:, 0:2, 0:8],
                                in0=gsel[:, sub * 8 + gi * 2: sub * 8 + gi * 2 + 2, :], scalar1=32768.0, scalar2=-32768.0,
                                op0=ALU.mult, op1=ALU.add), reads=[('gsel',), ('selsrc',)], writes=[('selsrc',)])
                        tp7 = ps[7][:, :].bitcast(BF16)
                        for gi in range(4):
                            S.op(PE, lambda: nc.tensor.transpose(tp7[0:96, gi * 128:(gi + 1) * 128], selsrc[:, gi, 0:96], ident_b[:, :]),
                                 reads=[('selsrc',), ('identb',)], writes=[PK(7)], inc=(gi == 3))
                        e = evac_eng()
                        S.op(e, copy_op(e, selT[0:96, :, sub * 128:(sub + 1) * 128], tp7[0:96, 0:512].rearrange("p (a b) -> p a b", a=4)),
                             reads=[PK(7)], writes=[('selT',)])

                if stage == 27 and g == 1:
                    return finish()
                nk = 4 * qj + 4
                W = (qj + 1) * 512
                hstate = {}
                info = []

                def emit_pv(i):
                    h, kt, qs, N, sl = info[i]
                    tzs_, pvb, pv3 = hstate[h]
                    nsub = N // 128
                    for s_ in range(nsub):
                        subq = (qs - q0) // 128 + s_
                        S.op(PE, lambda: nc.tensor.matmul(pv3[:, subq, :], pT[sl][:, s_ * 128:(s_ + 1) * 128], V[:, kt, h, :],
                                                          start=(kt == 0 and subq == 0), stop=(kt == 4 * qj + 3 and subq == 3),
                                                          skip_group_check=True),
                             reads=[('pT', sl), ('V', kt), ('Vones',)], writes=[PK(pvb)], inc=(s_ == nsub - 1))
                    if kt == nk - 1:
                        S.op(DVE, lambda: nc.vector.reciprocal(out=rden[:, 0:4], in_=pv3[:, :, 64]), reads=[PK(pvb)], writes=[('rden',)])
                        S.op(DVE, lambda: nc.vector.tensor_tensor(out=ytm[:, :, h * 64:(h + 1) * 64], in0=pv3[:, :, 0:64],
                                                                  in1=rden[:, 0:4].unsqueeze(2).to_broadcast([128, 4, 64]), op=ALU.mult),
                             reads=[PK(pvb), ('rden',)], writes=[('ytm',)])

                for h in range(8):
                    hp = h % 2
                    tzs = nxt('tz', 3)
                    S.dma('pool', out=tzb[tzs][:, 0:W],
                          in_=bass.AP(tz_scr, (g * 8 + h) * 128 * PITCH + 128, [[PITCH, 128], [1, W]]), writes=[('tz', tzs)])
                    pvb_ = 3 + nxt('pv', 2)
                    hstate[h] = (tzs, pvb_, ps[pvb_][:, 0:260].rearrange("p (s d) -> p s d", d=65))
                    for kt in range(nk):
                        Dd = q0 - kt * 128
                        if Dd >= 0:
                            qs, N, c0 = q0, 512, Dd
                        else:
                            qs, N, c0 = kt * 128, 512 + Dd, 0
                        sl = nxt('pslot', 4)
                        info.append((h, kt, qs, N, sl))
                        i = len(info) - 1
                        sbk = nxt('psS', 3)
                        S.op(PE, lambda: nc.tensor.matmul(ps[sbk][:, 0:N], qkT[hp * 64:(hp + 1) * 64, 4 + h // 2, kt * 128:(kt + 1) * 128],
                                                          qkT[hp * 64:(hp + 1) * 64, h // 2, qs:qs + N], start=True, stop=(g == 0)),
                             reads=[('qk', 4 + h // 2, kt // 4), ('qk', h // 2, qj)], writes=[PK(sbk)], inc=(g == 0))
                        if g == 1:
                            gi = h // 2
                            S.op(PE, lambda: nc.tensor.matmul(ps[sbk][:, 0:N], selhot[hp * 64:hp * 64 + 32, kt // 2, :],
                                                              selT[hp * 64:hp * 64 + 32, gi, qs - q0:qs - q0 + N], start=False, stop=True),
                                 reads=[('selhot',), ('selT',)], writes=[PK(sbk)], inc=True)
                        S.op(ACT, lambda: nc.scalar.activation(out=pexp[sl][:, 0:N], in_=ps[sbk][:, 0:N], func=AF.Exp, scale=0.125),
                             reads=[PK(sbk)], writes=[('pexp', sl)])
                        S.op(DVE, lambda: nc.vector.tensor_tensor(out=pT[sl][:, 0:N], in0=pexp[sl][:, 0:N], in1=tzb[tzs][:, c0:c0 + N], op=ALU.mult),
                             reads=[('pexp', sl), ('tz', tzs)], writes=[('pT', sl)])
                        if i > 2:
                            emit_pv(i - 3)
                emit_pv(len(info) - 3)
                emit_pv(len(info) - 2)
                emit_pv(len(info) - 1)

                if stage == 28 and g == 1 and qj == 0:
                    return finish()
                for sub in range(4):
                    S.op(ACT, lambda: nc.scalar.activation(out=junk[:, 0:512], in_=ytm[:, sub, :], func=AF.Square, accum_out=staty[:, 0, sub:sub + 1]),
                         reads=[('ytm',)], writes=[('junk',), ('ssy',)])
                rstd_from_ss(staty[:, 0, :], staty[:, 1, :], staty[:, 2, :], 512, [('ssy',)], ('rstdy',))
                for sub in range(4):
                    t = qj * 4 + sub
                    ys = nxt('ybf', 2)
                    S.op(POOL, lambda: nc.gpsimd.tensor_scalar(out=ybf[ys][:, :], in0=ytm[:, sub, :], scalar1=staty[:, 2, sub:sub + 1], scalar2=None, op0=ALU.mult),
                         reads=[('ytm',), ('rstdy',)], writes=[('ybf', ys)])
                    tpb = ps[5][:, :].bitcast(BF16)
                    for kc in range(4):
                        S.op(PE, lambda: nc.tensor.transpose(tpb[:, kc * 128:(kc + 1) * 128], ybf[ys][:, kc * 128:(kc + 1) * 128], ident_b[:, :]),
                             reads=[('ybf', ys), ('identb',)], writes=[PK(5)], inc=(kc == 3))
                    e = evac_eng()
                    S.op(e, copy_op(e, yT[:, g * 4:(g + 1) * 4, t * 128:(t + 1) * 128], tpb[:, 0:512].rearrange("p (a b) -> p a b", a=4)),
                         reads=[PK(5)], writes=[('yT', g, t)])
                if stage == 29 and g == 1 and qj == 0:
                    return finish()

            if stage == 25 and g == 0:
                return finish()
        if stage == 3:
            return finish()
        S.barrier()
        if stage == 37:
            return finish()
        so0, wo0 = load_w(wb_out, D, 0, 512)
        so1, wo1 = load_w(wb_out, D, 512, 512)
        for t in range(NT):
            xs = nxt('xt', 2)
            S.dma('pool', out=xt[xs][:, :], in_=x_d[b, t * 128:(t + 1) * 128, :], writes=[('xt', xs)])
            for nh in range(2):
                wsl, wvw = (so0, wo0) if nh == 0 else (so1, wo1)
                pb = nxt('psS', 3)
                for kc in range(KC):
                    S.op(PE, lambda: nc.tensor.matmul(ps[pb][:, :], yT[:, kc, t * 128:(t + 1) * 128], wvw[:, kc, :],
                                                      start=(kc == 0), stop=(kc == KC - 1)),
                         reads=[('ws', wsl), ('yT', 0, t), ('yT', 1, t)], writes=[PK(pb)], inc=(kc == KC - 1))
                S.op(DVE, lambda: nc.vector.tensor_tensor(out=H[:, t, nh * 512:(nh + 1) * 512], in0=ps[pb][:, :],
                                                          in1=xt[xs][:, nh * 512:(nh + 1) * 512], op=ALU.add),
                     reads=[PK(pb), ('xt', xs)], writes=[('H', t)])
            if stage != 35:
                norm_tile(H[:, t, :], [('H', t)], uT, t * 128, [('uT', t)], t)
        dump('h1', H[:, :, :], [('H', t) for t in range(NT)])
        if stage == 35:
            return finish()
        if stage == 4:
            return finish()
        S.barrier()

        for m in range(2):
            xs = nxt('xt', 2)
            S.dma('pool', out=xt[xs][:, :], in_=mem_d[b, m * 128:(m + 1) * 128, :], writes=[('xt', xs)])
            norm_tile(xt[xs][:, :], [('xt', xs)], mnT, m * 128, [('mnT', m)], m)
        S.op(POOL, lambda: nc.gpsimd.memset(vm[:, :, :, 256:257], 1.0), writes=[('vmones',)])
        wks = [load_w(wb_kv, 2 * D, j * 512, 512) for j in range(4)]
        for c in range(8):
            wsl, wvw = wks[c // 4]
            pb = nxt('psS', 3)
            for kc in range(KC):
                S.op(PE, lambda: nc.tensor.matmul(ps[pb][:, 0:NMEM], wvw[:, kc, (c % 4) * 128:(c % 4 + 1) * 128], mnT[:, kc, :],
                                                  start=(kc == 0), stop=(kc == KC - 1)),
                     reads=[('ws', wsl), ('mnT', 0), ('mnT', 1)], writes=[PK(pb)], inc=(kc == KC - 1))
            e = evac_eng()
            S.op(e, copy_op(e, kmTc[:, c, :], ps[pb][:, 0:NMEM]), reads=[PK(pb)], writes=[('kmTc', c)])
        for m in range(2):
            for nh in range(2):
                wsl, wvw = wks[2 + nh]
                pb = nxt('psS', 3)
                for kc in range(KC):
                    S.op(PE, lambda: nc.tensor.matmul(ps[pb][:, :], mnT[:, kc, m * 128:(m + 1) * 128], wvw[:, kc, :],
                                                      start=(kc == 0), stop=(kc == KC - 1)),
                         reads=[('ws', wsl), ('mnT', m)], writes=[PK(pb)], inc=(kc == KC - 1))
                e = evac_eng()
                S.op(e, copy_op(e, vm[:, m, 2 * nh:2 * nh + 2, 0:256], ps[pb][:, :].rearrange("p (a b) -> p a b", a=2)),
                     reads=[PK(pb), ('vmones',)], writes=[('vm', m)])
        wqs = [load_w(wb_q, D, j * 512, 512) for j in range(2)]
        wos = [load_w(wb_o, D, j * 512, 512) for j in range(2)]
        for qj in range(4):
            q0 = qj * 512
            for c in range(8):
                wsl, wvw = wqs[c // 4]
                pb = nxt('psS', 3)
                for kc in range(KC):
                    S.op(PE, lambda: nc.tensor.matmul(ps[pb][:, :], wvw[:, kc, (c % 4) * 128:(c % 4 + 1) * 128], uT[:, kc, q0:q0 + 512],
                                                      start=(kc == 0), stop=(kc == KC - 1)),
                         reads=[('ws', wsl)] + [('uT', 4 * qj + i) for i in range(4)], writes=[PK(pb)], inc=(kc == KC - 1))
                e = evac_eng()
                S.op(e, copy_op(e, qmT_t[:, c, :], ps[pb][:, :]), reads=[PK(pb)], writes=[('qmT', c)])
            for hm in range(4):
                for m in range(2):
                    pb = nxt('psS', 3)
                    for dc in range(2):
                        S.op(PE, lambda: nc.tensor.matmul(ps[pb][:, :], kmTc[:, 2 * hm + dc, m * 128:(m + 1) * 128], qmT_t[:, 2 * hm + dc, :],
                                                          start=(dc == 0), stop=(dc == 1)),
                             reads=[('kmTc', 2 * hm + dc), ('qmT', 2 * hm + dc)], writes=[PK(pb)], inc=(dc == 1))
                    S.op(ACT, lambda: nc.scalar.activation(out=pTc[m][:, :], in_=ps[pb][:, :], func=AF.Exp, scale=1.0 / 16.0),
                         reads=[PK(pb)], writes=[('pTc', m)])
                for sub in range(4):
                    for m in range(2):
                        S.op(PE, lambda: nc.tensor.matmul(ps[3 + sub // 2][:, (sub % 2) * 256:(sub % 2 + 1) * 256],
                                                          pTc[m][:, sub * 128:(sub + 1) * 128], vm[:, m, hm, 0:256],
                                                          start=(m == 0 and sub % 2 == 0), stop=(m == 1 and sub % 2 == 1), skip_group_check=True),
                             reads=[('pTc', m), ('vm', m)], writes=[PK(3 + sub // 2)], inc=False)
                        S.op(PE, lambda: nc.tensor.matmul(ps[6][:, sub * 8:(sub + 1) * 8], pTc[m][:, sub * 128:(sub + 1) * 128], ones8[:, :],
                                                          start=(m == 0 and sub == 0), stop=(m == 1 and sub == 3), skip_group_check=True),
                             reads=[('pTc', m), ('ones8',)], writes=[PK(6)], inc=(m == 1))
                S.op(DVE, lambda: nc.vector.reciprocal(out=rden[:, 4:8], in_=ps[6][:, 0:32].rearrange("p (a b) -> p a b", b=8)[:, :, 0]), reads=[PK(6)], writes=[('rdenc',)])
                for bk in range(2):
                    S.op(DVE, lambda: nc.vector.tensor_tensor(out=o_tm[:, 2 * bk:2 * bk + 2, hm * 256:(hm + 1) * 256],
                                                              in0=ps[3 + bk][:, :].rearrange("p (a b) -> p a b", a=2),
                                                              in1=rden[:, 4 + 2 * bk:6 + 2 * bk].unsqueeze(2).to_broadcast([128, 2, 256]), op=ALU.mult),
                         reads=[PK(3 + bk), ('rdenc',)], writes=[('o_tm', 2 * bk), ('o_tm', 2 * bk + 1)])
            for sub in range(4):
                t = qj * 4 + sub
                tpb = ps[5][:, :].bitcast(BF16)
                for kc in range(KC):
                    S.op(PE, lambda: nc.tensor.transpose(tpb[:, kc * 128:(kc + 1) * 128], o_tm[:, sub, kc * 128:(kc + 1) * 128], ident_b[:, :]),
                         reads=[('o_tm', sub), ('identb',)], writes=[PK(5)], inc=(kc == KC - 1))
                e = evac_eng()
                S.op(e, copy_op(e, oT_t[:, :, sub * 128:(sub + 1) * 128], tpb[:, 0:1024].rearrange("p (a b) -> p a b", a=KC)),
                     reads=[PK(5)], writes=[('oT', sub)])
                for nh in range(2):
                    wsl, wvw = wos[nh]
                    pb = nxt('psS', 3)
                    for kc in range(KC):
                        S.op(PE, lambda: nc.tensor.matmul(ps[pb][:, :], oT_t[:, kc, sub * 128:(sub + 1) * 128], wvw[:, kc, :],
                                                          start=(kc == 0), stop=(kc == KC - 1)),
                             reads=[('ws', wsl), ('oT', sub)], writes=[PK(pb)], inc=(kc == KC - 1))
                    S.op(DVE, lambda: nc.vector.tensor_tensor(out=H[:, t, nh * 512:(nh + 1) * 512], in0=ps[pb][:, :],
                                                              in1=H[:, t, nh * 512:(nh + 1) * 512], op=ALU.add),
                         reads=[PK(pb), ('H', t)], writes=[('H', t)])
                norm_tile(H[:, t, :], [('H', t)], uT, t * 128, [('uT', t)], t)
        dump('h2', H[:, :, :], [('H', t) for t in range(NT)])
        if stage == 5:
            return finish()
        S.barrier()

        for half in range(2):
            for cg in range(6):
                ncol = 512 if cg < 5 else 256
                sg, wg = load_w(wb_gate, DFF, cg * 512, ncol)
                su, wu = load_w(wb_up, DFF, cg * 512, ncol)
                for ci in range(ncol // 128):
                    c = cg * 4 + ci
                    for tt2 in range(2):
                        tt = half * 2 + tt2
                        pg = nxt('psG', 2)
                        pu = 2 + nxt('psU', 3)
                        rdk = [('uT', 4 * tt + i) for i in range(4)]
                        for kc in range(KC):
                            S.op(PE, lambda: nc.tensor.matmul(ps[pg][:, :], wg[:, kc, ci * 128:(ci + 1) * 128], uT[:, kc, tt * 512:(tt + 1) * 512],
                                                              start=(kc == 0), stop=(kc == KC - 1)),
                                 reads=[('ws', sg)] + rdk, writes=[PK(pg)], inc=(kc == KC - 1))
                        for kc in range(KC):
                            S.op(PE, lambda: nc.tensor.matmul(ps[pu][:, :], wu[:, kc, ci * 128:(ci + 1) * 128], uT[:, kc, tt * 512:(tt + 1) * 512],
                                                              start=(kc == 0), stop=(kc == KC - 1)),
                                 reads=[('ws', su)] + rdk, writes=[PK(pu)], inc=(kc == KC - 1))
                        sl = nxt('a_sb', 2)
                        S.op(ACT, lambda: nc.scalar.copy(out=a_sb[sl][:, 2:514], in_=ps[pg][:, :]), reads=[PK(pg)], writes=[('a_sb', sl)])
                        if tt == 0:
                            S.op(POOL, lambda: nc.gpsimd.memset(a_sb[sl][:, 0:2], 0.0), reads=[('a_sb', sl)], writes=[('a_sb', sl)])
                        elif tt2 == 1:
                            S.op(POOL, lambda: nc.gpsimd.tensor_copy(out=a_sb[sl][:, 0:2], in_=a_sb[1 - sl][:, 512:514]),
                                 reads=[('a_sb', 1 - sl), ('a_sb', sl)], writes=[('a_sb', sl)])
                        else:
                            S.op(POOL, lambda: nc.gpsimd.tensor_copy(out=a_sb[sl][:, 0:2], in_=halo[:, c, :]),
                                 reads=[('halo',), ('a_sb', sl)], writes=[('a_sb', sl)])
                        if tt == 1:
                            S.op(POOL, lambda: nc.gpsimd.tensor_copy(out=halo[:, c, :], in_=a_sb[sl][:, 512:514]),
                                 reads=[('a_sb', sl)], writes=[('halo',)])
                        S.op(ACT, lambda: nc.scalar.activation(out=t1[sl][:, :], in_=ps[pg][:, :], func=AF.Identity,
                                                               scale=convc[:, c, 2:3], bias=convc[:, c, 3:4]),
                             reads=[PK(pg), ('convc',)], writes=[('t1', sl)])
                        S.op(DVE, lambda: nc.vector.scalar_tensor_tensor(out=t1[sl][:, :], in0=a_sb[sl][:, 1:513], scalar=convc[:, c, 1:2],
                                                                         in1=t1[sl][:, :], op0=ALU.mult, op1=ALU.add),
                             reads=[('a_sb', sl), ('t1', sl), ('convc',)], writes=[('t1', sl)])
                        S.op(DVE, lambda: nc.vector.scalar_tensor_tensor(out=t1[sl][:, :], in0=a_sb[sl][:, 0:512], scalar=convc[:, c, 0:1],
                                                                         in1=t1[sl][:, :], op0=ALU.mult, op1=ALU.add),
                             reads=[('a_sb', sl), ('t1', sl), ('convc',)], writes=[('t1', sl)])
                        S.op(ACT, lambda: nc.scalar.activation(out=t1[sl][:, :], in_=t1[sl][:, :], func=AF.Silu),
                             reads=[('t1', sl)], writes=[('t1', sl)])
                        S.op(DVE, lambda: nc.vector.tensor_tensor(out=gT[:, c, tt2 * 512:(tt2 + 1) * 512], in0=t1[sl][:, :], in1=ps[pu][:, :], op=ALU.mult),
                             reads=[('t1', sl), PK(pu)], writes=[('gT', c, tt2)])
            for nh in range(2):
                wds = []
                for j in range(3):
                    nkc = 8 if j < 2 else 6
                    wds.append(load_w(wb_down, D, nh * 512, 512, row0=j * 1024, nkc=nkc))
                for tl in range(8):
                    t = half * 8 + tl
                    pb = 5 + nxt('psD', 3)
                    for kc in range(NFC):
                        wsl, wvw = wds[kc // 8]
                        S.op(PE, lambda: nc.tensor.matmul(ps[pb][:, :], gT[:, kc, tl * 128:(tl + 1) * 128], wvw[:, kc % 8, :],
                                                          start=(kc == 0), stop=(kc == NFC - 1)),
                             reads=[('ws', wsl), ('gT', kc, tl // 4)], writes=[PK(pb)], inc=(kc == NFC - 1))
                    S.op(DVE, lambda: nc.vector.tensor_tensor(out=H[:, t, nh * 512:(nh + 1) * 512], in0=ps[pb][:, :],
                                                              in1=H[:, t, nh * 512:(nh + 1) * 512], op=ALU.add),
                         reads=[PK(pb), ('H', t)], writes=[('H', t)])
                    if nh == 1:
                        S.op(ACT, lambda: nc.scalar.activation(out=junk[:, :], in_=H[:, t, :], func=AF.Square, accum_out=stat[:, 0, t:t + 1]),
                             reads=[('H', t)], writes=[('junk',), ('ss', t)])
                        rstd_from_ss(stat[:, 0, t:t + 1], stat[:, 1, t:t + 1], stat[:, 2, t:t + 1], D, [('ss', t)], ('rstd', t))
                        xs = nxt('xt', 2)
                        S.op(DVE, lambda: nc.vector.scalar_tensor_tensor(out=xt[xs][:, :], in0=H[:, t, :], scalar=stat[:, 2, t:t + 1], in1=gfin[:, :],
                                                                         op0=ALU.mult, op1=ALU.mult),
                             reads=[('H', t), ('rstd', t), ('gfin',)], writes=[('xt', xs)])
                        out_toks.append(S.dma('sp', out=out_d[b, t * 128:(t + 1) * 128, :], in_=xt[xs][:, :], reads=[('xt', xs)]))

    return finish()


_CACHE = {}


def kernel(**inputs):
    inp = {k: np.asarray(v) for k, v in inputs.items()}
    if 'nc' not in _CACHE:
        _CACHE['nc'] = build_program(NB_CORE)[0]
    nc = _CACHE['nc']
    consts = host_consts()
    shared = {
        "w_in": inp["w_in"].reshape(D, 3072), "w_out": inp["w_out"].reshape(D, D),
        "w_q_mem": inp["w_q_mem"].reshape(D, D), "w_kv_mem": inp["w_kv_mem"].reshape(D, 2 * D),
        "w_o_mem": inp["w_o_mem"].reshape(D, D), "w_gate": inp["w_gate"].reshape(D, DFF),
        "w_up": inp["w_up"].reshape(D, DFF), "w_down": inp["w_down"].reshape(DFF, D),
        "g_mix": inp["g_mix"].reshape(D), "g_cross": inp["g_cross"].reshape(D), "g_mem": inp["g_mem"].reshape(D),
        "g_ffn": inp["g_ffn"].reshape(D), "g_out_dil": inp["g_out_dil"].reshape(512),
        "g_out_moba": inp["g_out_moba"].reshape(512), "g_final": inp["g_final"].reshape(D),
        "rel_bias": inp["rel_bias"].reshape(16, 32), "conv_w": inp["conv_w"].reshape(3, DFF),
        "conv_b": inp["conv_b"].reshape(DFF),
    }
    shared = {k: np.ascontiguousarray(v, dtype=np.float32) for k, v in shared.items()}
    shared.update(consts)
    x = np.ascontiguousarray(inp["x"], dtype=np.float32)
    mem = np.ascontiguousarray(inp["mem"], dtype=np.float32)
    in_maps = []
    for i in range(NCORES):
        m = dict(shared)
        m["x"] = x[i * NB_CORE:(i + 1) * NB_CORE]
        m["mem"] = mem[i * NB_CORE:(i + 1) * NB_CORE]
        in_maps.append(m)
    res = run_bass_kernel_spmd(nc, in_maps, core_ids=list(range(NCORES)))
    return np.concatenate([np.asarray(r["out"]) for r in res.results], axis=0).astype(np.float32)
```
